# Optimizing a Trainium2 kernel written in Bass

```python
import jax
import jax.numpy as jnp
from jax import lax
import numpy as np

D_MODEL = 1024
BATCH = 8
SEQ = 8192
DEPTH = 1

HEAD_DIM = 64
N_HEADS_MOBA = 8
N_HEADS_SB = 8
W_MOBA = N_HEADS_MOBA * HEAD_DIM
W_SB = N_HEADS_SB * HEAD_DIM
MOBA_BLOCK = 256
MOBA_TOPK = 3
Q_BLOCK = 128
D_FF = 2816
CONV_WIDTH = 3
RMS_EPS = 1e-6
IN_SIZES = (W_MOBA, W_MOBA, W_MOBA, W_SB, W_SB, W_SB, D_MODEL, D_MODEL)
D_IN = 3 * W_MOBA + 3 * W_SB + 2 * D_MODEL

kernel_name = 'hybrid_moba_stickbreak_convffn'


def _rmsnorm(x, g):
    xf = x.astype(jnp.float32)
    y = xf * lax.rsqrt(jnp.mean(xf * xf, axis=-1, keepdims=True) + RMS_EPS)
    return (y * g.astype(jnp.float32)).astype(x.dtype)


def _alibi_slopes(n):
    return jnp.exp2(-8.0 * jnp.arange(1, n + 1, dtype=jnp.float32) / n)


def _split_columns(proj):
    parts = []
    start = 0
    for size in IN_SIZES:
        parts.append(proj[..., start:start + size])
        start += size
    return parts


def _heads(t, n_heads):
    b, t_len, _ = t.shape
    return t.reshape(b, t_len, n_heads, HEAD_DIM).transpose(0, 2, 1, 3)


def _merge_heads(t):
    b, h, t_len, d = t.shape
    return t.transpose(0, 2, 1, 3).reshape(b, t_len, h * d)


def _moba_attention(q, k, v):
    n_b, n_h, t_len, dh = q.shape
    n_blk = -(-t_len // MOBA_BLOCK)
    pad = n_blk * MOBA_BLOCK - t_len
    n_qb = t_len // Q_BLOCK
    n_sel = min(MOBA_TOPK, n_blk)
    scale = dh ** -0.5
    slopes = _alibi_slopes(n_h)[:, None, None]
    pad_cfg = ((0, 0), (0, 0), (0, pad), (0, 0))
    kf = jnp.pad(k.astype(jnp.float32), pad_cfg).reshape(n_b, n_h, n_blk, MOBA_BLOCK, dh)
    vf = jnp.pad(v.astype(jnp.float32), pad_cfg).reshape(n_b, n_h, n_blk, MOBA_BLOCK, dh)
    k_mean = jnp.mean(kf, axis=3)
    blk_ids = jnp.arange(n_blk)
    in_blk = jnp.arange(MOBA_BLOCK)
    head_ids = jnp.arange(n_h)[:, None, None]

    def per_seq(args):
        q1, kb, vb, km = args
        qc_all = q1.astype(jnp.float32).reshape(n_h, n_qb, Q_BLOCK, dh).transpose(1, 0, 2, 3)

        def per_chunk(cargs):
            i, qc = cargs
            q_pos = i * Q_BLOCK + jnp.arange(Q_BLOCK)
            own = (i * Q_BLOCK) // MOBA_BLOCK
            gate = jnp.einsum('hqd,hnd->hqn', qc, km)
            gate = jnp.where(blk_ids < own, gate, -jnp.inf)
            _, sel = lax.top_k(gate, n_sel)
            sel_valid = (sel < own)[..., None]
            k_sel = kb[head_ids, sel]
            v_sel = vb[head_ids, sel]
            sel_pos = sel[..., None] * MOBA_BLOCK + in_blk
            s_sel = jnp.einsum('hqd,hqjkd->hqjk', qc, k_sel) * scale
            s_sel = s_sel - slopes[..., None] * (q_pos[None, :, None, None] - sel_pos)
            s_sel = jnp.where(sel_valid, s_sel, -jnp.inf).reshape(n_h, Q_BLOCK, n_sel * MOBA_BLOCK)
            k_own = lax.dynamic_index_in_dim(kb, own, axis=1, keepdims=False)
            v_own = lax.dynamic_index_in_dim(vb, own, axis=1, keepdims=False)
            own_pos = own * MOBA_BLOCK + in_blk
            s_own = jnp.einsum('hqd,hkd->hqk', qc, k_own) * scale
            s_own = s_own - slopes * (q_pos[:, None] - own_pos[None, :])
            s_own = jnp.where(own_pos[None, None, :] <= q_pos[None, :, None], s_own, -jnp.inf)
            p = jax.nn.softmax(jnp.concatenate([s_sel, s_own], axis=-1), axis=-1)
            p_sel = p[..., :n_sel * MOBA_BLOCK].reshape(n_h, Q_BLOCK, n_sel, MOBA_BLOCK)
            p_own = p[..., n_sel * MOBA_BLOCK:]
            return (jnp.einsum('hqjk,hqjkd->hqd', p_sel, v_sel)
                    + jnp.einsum('hqk,hkd->hqd', p_own, v_own))

        out = lax.map(per_chunk, (jnp.arange(n_qb), qc_all))
        return out.transpose(1, 0, 2, 3).reshape(n_h, t_len, dh)

    out = lax.map(per_seq, (q, kf, vf, k_mean))
    return out.astype(q.dtype)


def _stick_breaking_attention(q, k, v):
    n_b, n_h, t_len, dh = q.shape
    n_qb = t_len // Q_BLOCK
    scale = dh ** -0.5
    key_pos = jnp.arange(t_len)

    def per_seq(args):
        q1, k1, v1 = args
        kf = k1.astype(jnp.float32)
        vf = v1.astype(jnp.float32)
        qc_all = q1.astype(jnp.float32).reshape(n_h, n_qb, Q_BLOCK, dh).transpose(1, 0, 2, 3)

        def per_chunk(cargs):
            i, qc = cargs
            q_pos = i * Q_BLOCK + jnp.arange(Q_BLOCK)
            z = jnp.einsum('hqd,hkd->hqk', qc, kf) * scale
            strict = (key_pos[None, :] < q_pos[:, None])[None]
            log_beta = jax.nn.log_sigmoid(z)
            log_1m_beta = jnp.where(strict, jax.nn.log_sigmoid(-z), 0.0)
            tail = lax.cumsum(log_1m_beta, axis=2, reverse=True) - log_1m_beta
            w = jnp.where(strict, jnp.exp(log_beta + tail), 0.0)
            return jnp.einsum('hqk,hkd->hqd', w, vf)

        out = lax.map(per_chunk, (jnp.arange(n_qb), qc_all))
        return out.transpose(1, 0, 2, 3).reshape(n_h, t_len, dh)

    out = lax.map(per_seq, (q, k, v))
    return out.astype(q.dtype)


def setup_inputs(seed: int = 0) -> dict:
    key = jax.random.key(seed)
    ks = jax.random.split(key, 13)
    f32 = jnp.float32

    def nrm(k, shape, fan_in):
        return jax.random.normal(k, shape, f32) * (fan_in ** -0.5)

    return {
        'x': jax.random.normal(ks[0], (BATCH, SEQ, D_MODEL), f32),
        'g_mix': 1.0 + 0.02 * jax.random.normal(ks[1], (DEPTH, D_MODEL), f32),
        'w_in': nrm(ks[2], (DEPTH, D_MODEL, D_IN), D_MODEL),
        'w_proj_moba': nrm(ks[3], (DEPTH, W_MOBA, D_MODEL), W_MOBA),
        'w_proj_sb': nrm(ks[4], (DEPTH, W_SB, D_MODEL), W_SB),
        'w_out': nrm(ks[5], (DEPTH, D_MODEL, D_MODEL), D_MODEL),
        'g_ffn': 1.0 + 0.02 * jax.random.normal(ks[6], (DEPTH, D_MODEL), f32),
        'w_up': nrm(ks[7], (DEPTH, D_MODEL, 2 * D_FF), D_MODEL),
        'conv_w': nrm(ks[8], (DEPTH, CONV_WIDTH, 1, 2 * D_FF), CONV_WIDTH),
        'conv_b': 0.01 * jax.random.normal(ks[9], (DEPTH, 2 * D_FF), f32),
        'w_down': nrm(ks[10], (DEPTH, D_FF, D_MODEL), D_FF),
        'g_final': 1.0 + 0.02 * jax.random.normal(ks[11], (D_MODEL,), f32),
    }


def reference(x, g_mix, w_in, w_proj_moba, w_proj_sb, w_out, g_ffn, w_up, conv_w, conv_b, w_down, g_final):
    for layer in range(DEPTH):
        h = _rmsnorm(x, g_mix[layer])
        proj = h @ w_in[layer]
        q_a, k_a, v_a, q_b, k_b, v_b, gate_a, gate_b = _split_columns(proj)
        y_a = _merge_heads(_moba_attention(_heads(q_a, N_HEADS_MOBA), _heads(k_a, N_HEADS_MOBA),
                                           _heads(v_a, N_HEADS_MOBA)))
        y_b = _merge_heads(_stick_breaking_attention(_heads(q_b, N_HEADS_SB), _heads(k_b, N_HEADS_SB),
                                                     _heads(v_b, N_HEADS_SB)))
        mixed = (jax.nn.sigmoid(gate_a) * (y_a @ w_proj_moba[layer])
                 + jax.nn.sigmoid(gate_b) * (y_b @ w_proj_sb[layer]))
        x = x + mixed @ w_out[layer]
        h = _rmsnorm(x, g_ffn[layer])
        u = h @ w_up[layer]
        u = lax.conv_general_dilated(u, conv_w[layer], window_strides=(1,),
                                     padding=[(CONV_WIDTH - 1, 0)],
                                     dimension_numbers=('NWC', 'WIO', 'NWC'),
                                     feature_group_count=2 * D_FF) + conv_b[layer]
        a = u[..., :D_FF]
        b = u[..., D_FF:]
        x = x + (jax.nn.silu(a) * b) @ w_down[layer]
    return _rmsnorm(x, g_final)
```

```python
import numpy as np
import ml_dtypes
from contextlib import ExitStack
import concourse.bass as bass
import concourse.mybir as mybir
from concourse.bass_utils import run_bass_kernel_spmd

F32 = mybir.dt.float32
BF16 = mybir.dt.bfloat16
AF = mybir.ActivationFunctionType
ALU = mybir.AluOpType
AX = mybir.AxisListType

D = 1024
DIN = 5120
DFF = 2816
NJ = DFF // 128
NH = 8
DH = 64
BIG = 32768.0
EPS = 1e-6
ENGS = ("sync", "scalar", "vector", "gpsimd", "tensor")


class Buf:
    __slots__ = ("name", "w", "r", "semkey")

    def __init__(self, name):
        self.name = name
        self.w = {}
        self.r = {}
        self.semkey = None


def _merge(d, s):
    for k, c in s.items():
        if d.get(k, 0) < c:
            d[k] = c


class Prog:
    def __init__(self, nc, es):
        self.nc = nc
        self.es = es
        self.sems = {}
        self.cnt = {}
        self.ops = {e: [] for e in ENGS}
        self.waited = {e: {} for e in ENGS}
        for e in ENGS:
            self._newsem(e)

    def _newsem(self, key):
        self.sems[key] = self.es.enter_context(self.nc.semaphore("s_" + key))
        self.cnt[key] = 0

    def _deps(self, eng, reads, writes, extra):
        deps = {}
        for b in reads:
            _merge(deps, b.w)
        for b in writes:
            _merge(deps, b.r)
            _merge(deps, b.w)
        for e in extra:
            if e:
                _merge(deps, e)
        waits = []
        wd = self.waited[eng]
        for k, c in deps.items():
            if eng == "tensor" and k == "tensor":
                continue
            if wd.get(k, 0) >= c:
                continue
            wd[k] = c
            waits.append((k, c))
        return waits

    def _update(self, ev, reads, writes):
        for b in writes:
            if b.r:
                b.r = {}
                b.w = {}
            _merge(b.w, ev)
        for b in reads:
            if b not in writes:
                _merge(b.r, ev)

    def op(self, eng, fn, reads=(), writes=(), extra=()):
        waits = self._deps(eng, reads, writes, extra)
        self.cnt[eng] += 1
        ev = {eng: self.cnt[eng]}
        self.ops[eng].append((waits, fn, (eng, 1)))
        self._update(ev, reads, writes)
        return ev

    def dma(self, fn, owner, reads=(), writes=(), extra=(), eng="sync"):
        if owner.semkey is None:
            owner.semkey = "d_" + owner.name
            self._newsem(owner.semkey)
        key = owner.semkey
        waits = self._deps(eng, reads, writes, extra)
        self.cnt[key] += 16
        ev = {key: self.cnt[key]}
        self.ops[eng].append((waits, fn, (key, 16)))
        self._update(ev, reads, writes)
        return ev

    def barrier(self):
        allc = {k: c for k, c in self.cnt.items() if c > 0}
        for eng in ENGS:
            waits = []
            wd = self.waited[eng]
            for k, c in allc.items():
                if k == eng:
                    continue
                if wd.get(k, 0) >= c:
                    continue
                wd[k] = c
                waits.append((k, c))
            if waits:
                self.ops[eng].append((waits, None, None))

    def emit(self):
        nc = self.nc
        sems = self.sems
        ops = self.ops
        self.ops = {e: [] for e in ENGS}
        with nc.Block() as block:
            def runner(name):
                def run(e):
                    for waits, fn, inc in ops[name]:
                        for k, c in waits:
                            e.wait_ge(sems[k], c)
                        if fn is not None:
                            inst = fn(e)
                            inst.then_inc(sems[inc[0]], inc[1])
                return run
            block.sync(runner("sync"))
            block.scalar(runner("scalar"))
            block.vector(runner("vector"))
            block.gpsimd(runner("gpsimd"))
            block.tensor(runner("tensor"))


def make_consts(T):
    bf = ml_dtypes.bfloat16
    NT = T // 128
    nblk = T // 256
    c = {}
    c["c_ident"] = np.eye(128, dtype=np.float32).astype(bf)
    j = np.arange(128)[:, None]
    k = np.arange(128)[None, :]
    c["c_negtri"] = np.where(j >= k, -1.0, 0.0).astype(bf)
    c["c_negones"] = np.full((128, 128), -1.0, np.float32).astype(bf)
    allneg = np.full((128, 128), -BIG, np.float32)
    zero = np.zeros((128, 128), np.float32)
    strict = np.where(j >= k, -BIG, 0.0).astype(np.float32)
    incl = np.where(j > k, -BIG, 0.0).astype(np.float32)
    c["c_mksb"] = np.concatenate([allneg] * 3 + [strict] + [zero] * 3, axis=1).astype(bf)
    c["c_mkmo"] = np.concatenate([allneg] * 3 + [incl] + [zero] * 3, axis=1).astype(bf)
    pos = np.arange(T)
    ind = (pos[None, :] // 256 == np.arange(nblk)[:, None]).astype(np.float32)
    c["c_ind"] = ind.astype(bf)
    slopes = 2.0 ** (-8.0 * np.arange(1, NH + 1) / NH)
    qal = np.zeros((NH, 4, T), np.float32)
    kal = np.zeros((NH, 4, T), np.float32)
    for h in range(NH):
        s = slopes[h]
        qal[h, 0] = -s * (pos % 128)
        qal[h, 1] = -s * 128.0 * (pos // 128)
        qal[h, 2] = 1.0
        qal[h, 3] = 1.0
        kal[h, 0] = 1.0
        kal[h, 1] = 1.0
        kal[h, 2] = s * (pos % 128)
        kal[h, 3] = s * 128.0 * (pos // 128)
    c["c_qal"] = qal.astype(bf)
    c["c_kal"] = kal.astype(bf)
    qt = np.arange(NT)[:, None]
    n = np.arange(nblk)[None, :]
    own = qt // 2
    pm = np.where(n < own, 0.0, -BIG).astype(np.float32)
    um = np.where(n < own, 0.0, np.where(n == own, BIG, -BIG)).astype(np.float32)
    c["c_pm"] = np.ascontiguousarray(np.broadcast_to(pm.reshape(1, -1), (128, NT * nblk))).astype(bf)
    c["c_um"] = np.ascontiguousarray(np.broadcast_to(um.reshape(1, -1), (128, NT * nblk))).astype(bf)
    return c


def build(T):
    NT = T // 128
    NCH = T // 512
    nblk = T // 256
    assert T % 512 == 0 and 8 <= nblk <= 32
    KR = 100

    nc = bass.Bass("TRN2", target_bir_lowering=False)

    def din(name, shape, dt=F32):
        return nc.dram_tensor(name, list(shape), dt, kind="ExternalInput").ap()

    x_d = din("x", [T, D])
    win_d = din("w_in", [D, DIN])
    wpm_d = din("w_pm", [512, D])
    wps_d = din("w_ps", [512, D])
    wout_d = din("w_out", [D, D])
    wup_d = din("w_up", [D, 2 * DFF])
    wdn_d = din("w_down", [DFF, D])
    gmix_d = din("g_mix", [128, 8])
    gffn_d = din("g_ffn", [128, 8])
    gfin_d = din("g_fin", [128, D])
    cw_d = din("cw", [128, 2 * NJ, 3])
    cb_d = din("cb", [128, 2 * NJ])
    ident_d = din("c_ident", [128, 128], BF16)
    negtri_d = din("c_negtri", [128, 128], BF16)
    negones_d = din("c_negones", [128, 128], BF16)
    mksb_d = din("c_mksb", [128, 896], BF16)
    mkmo_d = din("c_mkmo", [128, 896], BF16)
    ind_d = din("c_ind", [nblk, T], BF16)
    qal_d = din("c_qal", [NH, 4, T], BF16)
    kal_d = din("c_kal", [NH, 4, T], BF16)
    pm_d = din("c_pm", [128, NT * nblk], BF16)
    um_d = din("c_um", [128, NT * nblk], BF16)
    out_d = nc.dram_tensor("out", [T, D], F32, kind="ExternalOutput").ap()

    qt_s = nc.dram_tensor("qt_s", [16 * 64, T], BF16).ap()
    kt_s = nc.dram_tensor("kt_s", [16 * 64, T], BF16).ap()
    v_s = nc.dram_tensor("v_s", [16, T, 64], BF16).ap()
    g_s = nc.dram_tensor("g_s", [2 * D, T], BF16).ap()
    y_s = nc.dram_tensor("y_s", [D, T], BF16).ap()
    wup_s = nc.dram_tensor("wup_s", [2 * NJ, 128, 8, 128], BF16).ap()
    wdn_s = nc.dram_tensor("wdn_s", [DFF, D], BF16).ap()
    B_qt, B_kt, B_v, B_g, B_y, B_wup, B_wdn, B_out = (Buf(n) for n in
                                                     ("qt_s", "kt_s", "v_s", "g_s", "y_s", "wup_s", "wdn_s", "out"))

    with ExitStack() as es:
        P = Prog(nc, es)

        def sbt(st, name, shape, dt):
            return st.enter_context(nc.sbuf_tensor("sb_" + name, list(shape), dt))

        pb = [es.enter_context(nc.psum_tensor(f"pb{i}", [128, 512], F32)) for i in range(8)]
        PB = [Buf(f"pb{i}") for i in range(8)]

        ident = sbt(es, "ident", [128, 128], BF16)
        negtri = sbt(es, "negtri", [128, 128], BF16)
        negones = sbt(es, "negones", [128, 128], BF16)
        negh = sbt(es, "negh", [128, 4], F32)
        ones32 = sbt(es, "ones32", [128, 128], F32)
        B_const = Buf("const")

        P.dma(lambda e: e.dma_start(out=ident[:], in_=ident_d), B_const, writes=[B_const])
        P.dma(lambda e: e.dma_start(out=negtri[:], in_=negtri_d), B_const, writes=[B_const])
        P.dma(lambda e: e.dma_start(out=negones[:], in_=negones_d), B_const, writes=[B_const])
        P.op("vector", lambda e: e.memset(negh[:], -0.5), writes=[B_const])
        P.op("vector", lambda e: e.memset(ones32[:], 1.0), writes=[B_const])

        def rstd_from_ss(ss, ms, rstd, B_ss, B_ms, B_rstd, n):
            P.op("vector", lambda e: e.tensor_scalar(out=ms[:, 0:n], in0=ss[:, 0:n], scalar1=1.0 / D, scalar2=EPS,
                                                     op0=ALU.mult, op1=ALU.add),
                 reads=[B_ss], writes=[B_ms])
            P.op("gpsimd", lambda e: e.tensor_tensor(out=rstd[:, 0:n], in0=ms[:, 0:n], in1=negh[:, 0:n], op=ALU.pow),
                 reads=[B_ms, B_const], writes=[B_rstd])

        with ExitStack() as sa:
            winb = sbt(sa, "winb", [128, 8, DIN], BF16)
            wst = [sbt(sa, f"wst{i}", [128, 2560], F32) for i in range(2)]
            B_wst = [Buf(f"wst{i}") for i in range(2)]
            B_winb = Buf("winb")
            gmix = sbt(sa, "gmix", [128, 8], F32)
            B_gmix = Buf("gmix")
            xt = [sbt(sa, f"xt{i}", [128, D], F32) for i in range(8)]
            B_xt = [Buf(f"xt{i}") for i in range(8)]
            junk = sbt(sa, "junk", [128, D], BF16)
            B_junk = Buf("junk")
            ss = [sbt(sa, f"ss{i}", [128, 4], F32) for i in range(2)]
            ms = [sbt(sa, f"ms{i}", [128, 4], F32) for i in range(2)]
            rstd = [sbt(sa, f"rstd{i}", [128, 4], F32) for i in range(2)]
            B_ss = [Buf(f"ss{i}") for i in range(2)]
            B_ms = [Buf(f"ms{i}") for i in range(2)]
            B_rstd = [Buf(f"rstd{i}") for i in range(2)]
            hn = [sbt(sa, f"hn{i}", [128, D], BF16) for i in range(2)]
            B_hn = [Buf(f"hn{i}") for i in range(2)]
            hT = [sbt(sa, f"hT{i}", [128, 8, 512], BF16) for i in range(2)]
            B_hT = [Buf(f"hT{i}") for i in range(2)]
            stg = [sbt(sa, f"stg{i}", [128, 4, 512], BF16) for i in range(4)]
            B_stg = [Buf(f"stg{i}") for i in range(4)]

            P.dma(lambda e: e.dma_start(out=gmix[:], in_=gmix_d), B_gmix, writes=[B_gmix])
            pi = 0
            B_winh = [Buf("winb0"), Buf("winb1")]
            for hf in range(2):
                for j in range(8):
                    b = pi % 2
                    P.dma(lambda e, j=j, hf=hf, b=b: e.dma_start(
                        out=wst[b][:], in_=win_d[j * 128:(j + 1) * 128, hf * 2560:(hf + 1) * 2560]),
                        B_wst[b], writes=[B_wst[b]])
                    if pi % 2 == 0:
                        P.op("vector", lambda e, j=j, hf=hf, b=b: e.tensor_scalar(
                            out=winb[:, j, hf * 2560:(hf + 1) * 2560], in0=wst[b][:], scalar1=gmix[:, j:j + 1],
                            scalar2=None, op0=ALU.mult),
                            reads=[B_wst[b], B_gmix], writes=[B_winh[hf]])
                    else:
                        P.op("scalar", lambda e, j=j, hf=hf, b=b: e.activation(
                            out=winb[:, j, hf * 2560:(hf + 1) * 2560], in_=wst[b][:], func=AF.Copy, scale=gmix[:, j:j + 1]),
                            reads=[B_wst[b], B_gmix], writes=[B_winh[hf]])
                    pi += 1

            pbTa = [pb[6][:].bitcast(BF16), pb[7][:].bitcast(BF16)]

            def load_x(c):
                for s in range(4):
                    xi = (c % 2) * 4 + s
                    t0 = c * 512 + s * 128
                    P.dma(lambda e, xi=xi, t0=t0: e.dma_start(out=xt[xi][:], in_=x_d[t0:t0 + 128, :]),
                          B_xt[xi], writes=[B_xt[xi]])

            load_x(0)
            stg_i = 0
            evac_i = 0
            for c in range(NCH):
                cb_ = c % 2
                if c + 1 < NCH:
                    load_x(c + 1)
                for s in range(4):
                    xi = cb_ * 4 + s
                    P.op("scalar", lambda e, xi=xi, s=s, cb_=cb_: e.activation(
                        out=junk[:], in_=xt[xi][:], func=AF.Square, accum_out=ss[cb_][:, s:s + 1]),
                        reads=[B_xt[xi]], writes=[B_junk, B_ss[cb_]])
                rstd_from_ss(ss[cb_], ms[cb_], rstd[cb_], B_ss[cb_], B_ms[cb_], B_rstd[cb_], 4)
                for s in range(4):
                    xi = cb_ * 4 + s
                    hb = s % 2
                    P.op("vector", lambda e, xi=xi, s=s, hb=hb, cb_=cb_: e.tensor_scalar(
                        out=hn[hb][:], in0=xt[xi][:], scalar1=rstd[cb_][:, s:s + 1], scalar2=None, op0=ALU.mult),
                        reads=[B_xt[xi], B_rstd[cb_]], writes=[B_hn[hb]])

                    def tr(e, hb=hb):
                        inst = None
                        for j in range(8):
                            inst = e.transpose(out=pbTa[hb][:, j * 128:(j + 1) * 128], in_=hn[hb][:, j * 128:(j + 1) * 128],
                                               identity=ident[:])
                        return inst
                    P.op("tensor", tr, reads=[B_hn[hb], B_const], writes=[PB[6 + hb]])
                    P.op("vector", lambda e, s=s, cb_=cb_, hb=hb: e.tensor_copy(
                        out=hT[cb_][:, :, s * 128:(s + 1) * 128],
                        in_=pbTa[hb].rearrange("p (j t) -> p j t", j=8)),
                        reads=[PB[6 + hb]], writes=[B_hT[cb_]])

                for grp in range(10):
                    col0 = grp * 512
                    if grp in (2, 5):
                        sb_ = stg_i % 4
                        stg_i += 1
                        for s in range(4):
                            bk = evac_i % 6
                            evac_i += 1

                            def mmv(e, s=s, bk=bk, col0=col0, cb_=cb_):
                                inst = None
                                for j in range(8):
                                    inst = e.matmul(pb[bk][:], lhsT=hT[cb_][:, j, s * 128:(s + 1) * 128],
                                                    rhs=winb[:, j, col0:col0 + 512], start=(j == 0), stop=(j == 7))
                                return inst
                            P.op("tensor", mmv, reads=[B_hT[cb_], B_winh[grp // 5]], writes=[PB[bk]])
                            P.op("vector", lambda e, s=s, bk=bk, sb_=sb_: e.tensor_copy(out=stg[sb_][:, s, :], in_=pb[bk][:]),
                                 reads=[PB[bk]], writes=[B_stg[sb_]])
                        hg0 = 0 if grp == 2 else 8
                        for s in range(4):
                            t0 = c * 512 + s * 128
                            P.dma(lambda e, s=s, sb_=sb_, hg0=hg0, t0=t0: e.dma_start(
                                out=v_s[hg0:hg0 + 8, t0:t0 + 128, :].rearrange("h t d -> t h d"),
                                in_=stg[sb_][:, s, :].rearrange("p (h d) -> p h d", h=8)),
                                B_stg[sb_], reads=[B_stg[sb_]], writes=[B_v])
                    else:
                        sb_ = stg_i % 4
                        stg_i += 1
                        for q in range(4):
                            bk = evac_i % 6
                            evac_i += 1
                            cc = col0 + q * 128

                            def mmf(e, bk=bk, cc=cc, cb_=cb_):
                                inst = None
                                for j in range(8):
                                    inst = e.matmul(pb[bk][:], lhsT=winb[:, j, cc:cc + 128], rhs=hT[cb_][:, j, :],
                                                    start=(j == 0), stop=(j == 7))
                                return inst
                            P.op("tensor", mmf, reads=[B_hT[cb_], B_winh[grp // 5]], writes=[PB[bk]])
                            if grp >= 6:
                                P.op("scalar", lambda e, bk=bk, sb_=sb_, q=q: e.activation(
                                    out=stg[sb_][:, q, :], in_=pb[bk][:], func=AF.Sigmoid),
                                    reads=[PB[bk]], writes=[B_stg[sb_]])
                            elif grp in (0, 3):
                                P.op("scalar", lambda e, bk=bk, sb_=sb_, q=q: e.activation(
                                    out=stg[sb_][:, q, :], in_=pb[bk][:], func=AF.Copy, scale=0.125),
                                    reads=[PB[bk]], writes=[B_stg[sb_]])
                            else:
                                P.op("vector", lambda e, bk=bk, sb_=sb_, q=q: e.tensor_copy(
                                    out=stg[sb_][:, q, :], in_=pb[bk][:]),
                                    reads=[PB[bk]], writes=[B_stg[sb_]])
                        t0 = c * 512
                        if grp in (0, 3):
                            dst, r0, Bd = qt_s, (0 if grp == 0 else 512), B_qt
                        elif grp in (1, 4):
                            dst, r0, Bd = kt_s, (0 if grp == 1 else 512), B_kt
                        else:
                            dst, r0, Bd = g_s, (grp - 6) * 512, B_g
                        P.dma(lambda e, dst=dst, r0=r0, t0=t0, sb_=sb_: e.dma_start(
                            out=dst[r0:r0 + 512, t0:t0 + 512].rearrange("(g p) t -> p g t", p=128),
                            in_=stg[sb_][:]),
                            B_stg[sb_], reads=[B_stg[sb_]], writes=[Bd])
            P.barrier()
            P.emit()

        wpm = sbt(es, "wpm", [128, 4, D], BF16)
        wps = sbt(es, "wps", [128, 4, D], BF16)
        wout = sbt(es, "wout", [128, 8, D], BF16)
        B_wres = Buf("wres")
        gffn = sbt(es, "gffn", [128, 8], F32)
        gfin = sbt(es, "gfin", [128, D], F32)
        cw = sbt(es, "cw", [128, 2 * NJ, 3], F32)
        cbt = sbt(es, "cbt", [128, 2 * NJ], F32)
        halo = sbt(es, "halo", [128, 2 * NJ, 2], F32)
        B_halo = Buf("halo")
        B_cc = Buf("constC")
        P.dma(lambda e: e.dma_start(out=gffn[:], in_=gffn_d), B_cc, writes=[B_cc])
        P.dma(lambda e: e.dma_start(out=gfin[:], in_=gfin_d), B_cc, writes=[B_cc])
        P.dma(lambda e: e.dma_start(out=cw[:], in_=cw_d), B_cc, writes=[B_cc])
        P.dma(lambda e: e.dma_start(out=cbt[:], in_=cb_d), B_cc, writes=[B_cc])
        P.op("vector", lambda e: e.memset(halo[:], 0.0), writes=[B_halo])


        with ExitStack() as sb:
            QA = [sbt(sb, f"QA{i}", [128, T], BF16) for i in range(2)]
            KA = [sbt(sb, f"KA{i}", [128, T], BF16) for i in range(2)]
            VA = [sbt(sb, f"VA{i}", [128, NT + 1, 65], BF16) for i in range(2)]
            VAf = [VA[i][:].rearrange("p t c -> p (t c)") for i in range(2)]
            B_QAq = [Buf(f"QAq{i}") for i in range(2)]
            B_QAs = [Buf(f"QAs{i}") for i in range(2)]
            B_KA = [Buf(f"KA{i}") for i in range(2)]
            B_VA = [Buf(f"VA{i}") for i in range(2)]
            mksb = sbt(sb, "mksb", [128, 896], BF16)
            mkmo = sbt(sb, "mkmo", [128, 896], BF16)
            pmt = sbt(sb, "pmt", [128, NT * nblk], BF16)
            umt = sbt(sb, "umt", [128, NT * nblk], BF16)
            B_cb = Buf("constB")
            e32 = [sbt(sb, f"e32_{i}", [128, 512], F32) for i in range(3)]
            B_e32 = [Buf(f"e32_{i}") for i in range(3)]
            Lb = [sbt(sb, f"Lb{i}", [128, 512], BF16) for i in range(4)]
            B_Lb = [Buf(f"Lb{i}") for i in range(4)]
            Rb = [sbt(sb, f"Rb{i}", [128, 512], BF16) for i in range(4)]
            B_Rb = [Buf(f"Rb{i}") for i in range(4)]
            wb = [sbt(sb, f"wb{i}", [128, 512], BF16) for i in range(4)]
            B_wb = [Buf(f"wb{i}") for i in range(4)]
            yst = [sbt(sb, f"yst{i}", [64, 512], BF16) for i in range(2)]
            B_yst = [Buf(f"yst{i}") for i in range(2)]
            rden = sbt(sb, "rden", [128, 512], F32)
            B_rden = Buf("rden")
            bcs = sbt(sb, "bcs", [64, 512], F32)
            B_bcs = Buf("bcs")
            km32 = sbt(sb, "km32", [64, nblk], F32)
            kmb = sbt(sb, "kmb", [128, nblk], BF16)
            B_km32 = Buf("km32")
            B_kmb = Buf("kmb")
            gm32 = sbt(sb, "gm32", [128, 16 * nblk], F32)
            B_gm32 = Buf("gm32")
            m8 = sbt(sb, "m8", [128, 16, 8], F32)
            B_m8 = Buf("m8")
            sel = sbt(sb, "sel", [128, 16 * nblk], F32)
            B_sel = Buf("sel")
            sel2 = sbt(sb, "sel2", [128, 16 * nblk], F32)
            B_sel2 = Buf("sel2")
            selT = sbt(sb, "selT", [128, 16, 96], BF16)
            B_selT = Buf("selT")

            cst = [sbt(sb, f"cst{i}", [128, 1408], F32) for i in range(2)]
            cbf = [sbt(sb, f"cbf{i}", [128, 1408], BF16) for i in range(2)]
            B_cst = [Buf(f"cst{i}") for i in range(2)]
            B_cbf = [Buf(f"cbf{i}") for i in range(2)]
            ci = [0]

            def conv_piece(src_ap, a, n, scale_ap=None, dst_fn=None, dst_sbuf_ap=None, Bdst=None):
                ncols = a * n
                b = ci[0] % 2
                ci[0] += 1
                dstv = cst[b][:, 0:ncols] if a == 1 else cst[b][:, 0:ncols].rearrange("p (a n) -> p a n", a=a)
                P.dma(lambda e: e.dma_start(out=dstv, in_=src_ap), B_cst[b], writes=[B_cst[b]])
                if dst_fn is not None:
                    if scale_ap is not None:
                        P.op("vector", lambda e: e.tensor_scalar(out=cbf[b][:, 0:ncols], in0=cst[b][:, 0:ncols],
                                                                 scalar1=scale_ap, scalar2=None, op0=ALU.mult),
                             reads=[B_cst[b], B_cc], writes=[B_cbf[b]])
                    else:
                        P.op("vector", lambda e: e.tensor_copy(out=cbf[b][:, 0:ncols], in_=cst[b][:, 0:ncols]),
                             reads=[B_cst[b]], writes=[B_cbf[b]])
                    dst_fn(b)
                else:
                    P.op("vector", lambda e: e.tensor_copy(out=dst_sbuf_ap, in_=cst[b][:, 0:ncols]),
                         reads=[B_cst[b]], writes=[Bdst])

            def conv_gen():
                for k in range(8):
                    for q4 in range(4):
                        def dst_fn(b, k=k, q4=q4):
                            j0 = q4 * 11
                            P.dma(lambda e: e.dma_start(
                                out=wup_s[j0:j0 + 11, :, k, :].rearrange("j p c -> p j c"),
                                in_=cbf[b][:, 0:1408].rearrange("p (j c) -> p j c", c=128)),
                                B_cbf[b], reads=[B_cbf[b]], writes=[B_wup])
                        conv_piece(wup_d[k * 128:(k + 1) * 128, q4 * 1408:(q4 + 1) * 1408], 1, 1408,
                                   scale_ap=gffn[:, k:k + 1], dst_fn=dst_fn)
                        yield
                for j in range(NJ):
                    def dst_fn(b, j=j):
                        P.dma(lambda e: e.dma_start(out=wdn_s[j * 128:(j + 1) * 128, :], in_=cbf[b][:, 0:1024]),
                              B_cbf[b], reads=[B_cbf[b]], writes=[B_wdn])
                    conv_piece(wdn_d[j * 128:(j + 1) * 128, :], 1, 1024, dst_fn=dst_fn)
                    yield
                for k in range(4):
                    conv_piece(wpm_d[k * 128:(k + 1) * 128, :], 1, 1024, dst_sbuf_ap=wpm[:, k, :], Bdst=B_wres)
                    yield
                    conv_piece(wps_d[k * 128:(k + 1) * 128, :], 1, 1024, dst_sbuf_ap=wps[:, k, :], Bdst=B_wres)
                    yield
                for k in range(8):
                    conv_piece(wout_d[k * 128:(k + 1) * 128, :], 1, 1024, dst_sbuf_ap=wout[:, k, :], Bdst=B_wres)
                    yield

            P.dma(lambda e: e.dma_start(out=mksb[:], in_=mksb_d), B_cb, writes=[B_cb])
            P.dma(lambda e: e.dma_start(out=mkmo[:], in_=mkmo_d), B_cb, writes=[B_cb])
            P.dma(lambda e: e.dma_start(out=pmt[:], in_=pm_d), B_cb, writes=[B_cb])
            P.dma(lambda e: e.dma_start(out=umt[:], in_=um_d), B_cb, writes=[B_cb])
            P.op("vector", lambda e: e.memset(selT[:], 0.0), writes=[B_selT])
            P.op("vector", lambda e: e.memset(kmb[:], 0.0), writes=[B_kmb])
            P.op("vector", lambda e: e.memset(rden[:], 0.0), writes=[B_rden])
            for b in range(2):
                P.op("gpsimd", lambda e, b=b: e.memset(QA[b][64:128, :], 0.0), writes=[B_QAs[b], B_QAq[b]])
                P.op("gpsimd", lambda e, b=b: e.memset(KA[b][64:128, :], 0.0), writes=[B_KA[b]])
                P.op("vector", lambda e, b=b: e.memset(VA[b][:, NT, :], 0.0), writes=[B_VA[b]])
                P.op("vector", lambda e, b=b: e.memset(VA[b][:, 0:NT, 64:65], 1.0), writes=[B_VA[b]])
                P.dma(lambda e, b=b: e.dma_start(out=KA[b][64:64 + nblk, :], in_=ind_d), B_KA[b], writes=[B_KA[b]])

            heads = [("sb", 0)] + [("moba", h) for h in range(NH)] + [("sb", h) for h in range(1, NH)]

            def load_head(hi):
                kind, h = heads[hi]
                b = hi % 2
                hg = h if kind == "moba" else 8 + h
                if kind == "sb" and hi in (NH + 1, NH + 2):
                    P.op("gpsimd", lambda e: e.memset(QA[b][64:128, :], 0.0), writes=[B_QAs[b], B_QAq[b]])
                P.dma(lambda e: e.dma_start(out=QA[b][0:64, :], in_=qt_s[hg * 64:(hg + 1) * 64, :]),
                      B_QAq[b], reads=[B_qt], writes=[B_QAq[b]])
                P.dma(lambda e: e.dma_start(out=KA[b][0:64, :], in_=kt_s[hg * 64:(hg + 1) * 64, :]),
                      B_KA[b], reads=[B_kt], writes=[B_KA[b]])
                step = 8
                for t0 in range(0, NT, step):
                    P.dma(lambda e, t0=t0: e.dma_start(
                        out=VA[b][:, t0:t0 + step, 0:64],
                        in_=v_s[hg, t0 * 128:(t0 + step) * 128, :].rearrange("(t p) d -> p t d", p=128)),
                        B_VA[b], reads=[B_v], writes=[B_VA[b]])
                if kind == "moba":
                    P.dma(lambda e: e.dma_start(out=QA[b][96:100, :], in_=qal_d[h]), B_QAq[b], writes=[B_QAq[b]])
                    P.dma(lambda e: e.dma_start(out=KA[b][96:100, :], in_=kal_d[h]), B_KA[b], writes=[B_KA[b]])

            def selection(hi):
                kind, h = heads[hi]
                b = hi % 2
                for n0 in range(0, nblk, 4):
                    P.op("vector", lambda e, n0=n0: e.tensor_reduce(
                        out=km32[:, n0:n0 + 4],
                        in_=KA[b][0:64, n0 * 256:(n0 + 4) * 256].rearrange("p (n k) -> p n k", k=256),
                        axis=AX.X, op=ALU.add),
                        reads=[B_KA[b]], writes=[B_km32])
                    yield
                P.op("vector", lambda e: e.tensor_scalar(out=kmb[0:64, :], in0=km32[:, :], scalar1=1.0 / 256, scalar2=None,
                                                         op0=ALU.mult),
                     reads=[B_km32], writes=[B_kmb])
                for q0 in range(0, NT, 16):
                    nq = min(16, NT - q0)

                    def gate_mm(e, q0=q0, nq=nq):
                        inst = None
                        for i in range(nq):
                            qt_ = q0 + i
                            inst = e.matmul(pb[6][:, i * nblk:(i + 1) * nblk], lhsT=QA[b][:, qt_ * 128:(qt_ + 1) * 128],
                                            rhs=kmb[:, :], start=True, stop=True)
                        return inst
                    P.op("tensor", gate_mm, reads=[B_QAq[b], B_QAs[b], B_kmb], writes=[PB[6]])
                    P.op("vector", lambda e, q0=q0, nq=nq: e.tensor_tensor(
                        out=gm32[:, 0:nq * nblk], in0=pb[6][:, 0:nq * nblk], in1=pmt[:, q0 * nblk:(q0 + nq) * nblk],
                        op=ALU.add),
                        reads=[PB[6], B_cb], writes=[B_gm32])
                    yield
                    for i in range(nq):
                        P.op("vector", lambda e, i=i: e.max(out=m8[:, i, :], in_=gm32[:, i * nblk:(i + 1) * nblk]),
                             reads=[B_gm32], writes=[B_m8])
                        if i % 4 == 3:
                            yield
                    for i in range(nq):
                        P.op("vector", lambda e, i=i: e.tensor_scalar(
                            out=sel[:, i * nblk:(i + 1) * nblk], in0=gm32[:, i * nblk:(i + 1) * nblk],
                            scalar1=m8[:, i, 2:3], scalar2=1.0, op0=ALU.is_ge, op1=ALU.subtract),
                            reads=[B_gm32, B_m8], writes=[B_sel])
                        if i % 4 == 3:
                            yield
                    P.op("vector", lambda e, q0=q0, nq=nq: e.scalar_tensor_tensor(
                        out=sel2[:, 0:nq * nblk], in0=sel[:, 0:nq * nblk], scalar=BIG,
                        in1=umt[:, q0 * nblk:(q0 + nq) * nblk], op0=ALU.mult, op1=ALU.add),
                        reads=[B_sel, B_cb], writes=[B_sel2])
                    P.op("vector", lambda e, nq=nq: e.tensor_scalar(
                        out=selT[:, 0:nq, 64:64 + nblk], in0=sel2[:, 0:nq * nblk].rearrange("p (i n) -> p i n", n=nblk),
                        scalar1=0.0, scalar2=None, op0=ALU.min),
                        reads=[B_sel2], writes=[B_selT])
                    yield
                    yield
                    for g0 in range(0, nq, 4):
                        def tr_mm(e, g0=g0):
                            inst = None
                            for i in range(4):
                                inst = e.matmul(pb[7][0:96, i * 128:(i + 1) * 128], lhsT=selT[:, g0 + i, 0:96],
                                                rhs=ident[:], start=True, stop=True)
                            return inst
                        P.op("tensor", tr_mm, reads=[B_selT, B_const], writes=[PB[7]])
                        c0 = (q0 + g0) * 128
                        P.op("vector", lambda e, c0=c0: e.tensor_copy(out=QA[b][64:96, c0:c0 + 512], in_=pb[7][64:96, :]),
                             reads=[PB[7]], writes=[B_QAs[b]])
                        yield

            ycnt = [0]

            def moba_head(hi, gens):
                kind, h = heads[hi]
                b = hi % 2
                units = [(c, i) for c in range(NCH) for i in range(4 * c + 4)]
                N = len(units)
                LA = 3
                pslot = {}
                pending = []
                for n in range(N + LA):
                    if n % 5 == 0 and n >= 64:
                        for g in gens:
                            next(g, None)
                    while pending and pending[0][0] <= n:
                        pending.pop(0)[1]()
                    if n < N:
                        c, i = units[n]
                        bk = n % 4
                        diag = i >= 4 * c
                        lo = 128 * (i - 4 * c) if diag else 0

                        def s1(e, c=c, i=i, bk=bk, diag=diag, lo=lo):
                            inst = e.matmul(pb[bk][:, lo:512], lhsT=KA[b][:, i * 128:(i + 1) * 128],
                                            rhs=QA[b][:, c * 512 + lo:(c + 1) * 512], start=True, stop=not diag)
                            if diag:
                                inst = e.matmul(pb[bk][:, lo:512], lhsT=ident[:], rhs=mkmo[:, 384:384 + 512 - lo],
                                                start=False, stop=True)
                            return inst
                        P.op("tensor", s1, reads=[B_KA[b], B_QAq[b], B_QAs[b], B_const, B_cb], writes=[PB[bk]])
                        ps_ = n % 4
                        pslot[n] = ps_
                        P.op("scalar", lambda e, bk=bk, ps_=ps_, lo=lo: e.activation(
                            out=wb[ps_][:, lo:512], in_=pb[bk][:, lo:512], func=AF.Exp),
                            reads=[PB[bk]], writes=[B_wb[ps_]])
                    m = n - LA
                    if m >= 0:
                        c, i = units[m]
                        yb = 4 + (c % 2)
                        ps_ = pslot[m]
                        last = (i == 4 * c + 3)
                        lo = 128 * (i - 4 * c) if i >= 4 * c else 0
                        P.op("tensor", lambda e, c=c, i=i, yb=yb, ps_=ps_, last=last, lo=lo: e.matmul(
                            pb[yb][:, lo:512], lhsT=VAf[b][:, i * 65:i * 65 + 128], rhs=wb[ps_][:, lo:512],
                            start=(i == 0), stop=last, skip_group_check=True),
                            reads=[B_VA[b], B_wb[ps_]], writes=[PB[yb]])
                        if last:
                            while pending:
                                pending.pop(0)[1]()
                            ys = ycnt[0] % 2
                            ycnt[0] += 1
                            P.op("vector", lambda e, yb=yb: e.reciprocal(out=rden[64:65, :], in_=pb[yb][64:65, :]),
                                 reads=[PB[yb]], writes=[B_rden])

                            def finish(yb=yb, ys=ys, c=c):
                                P.op("tensor", lambda e: e.matmul(pb[6][:, :], lhsT=ones32[:, :], rhs=rden[:, :],
                                                                  start=True, stop=True),
                                     reads=[B_rden, B_const], writes=[PB[6]])
                                P.op("vector", lambda e: e.tensor_copy(out=bcs[:, :], in_=pb[6][0:64, :]),
                                     reads=[PB[6]], writes=[B_bcs])
                                P.op("vector", lambda e: e.tensor_tensor(
                                    out=yst[ys][:, :], in0=pb[yb][0:64, :], in1=bcs[:, :], op=ALU.mult),
                                    reads=[PB[yb], B_bcs], writes=[B_yst[ys]])
                                P.dma(lambda e: e.dma_start(
                                    out=y_s[h * 64:(h + 1) * 64, c * 512:(c + 1) * 512], in_=yst[ys][:, :]),
                                    B_yst[ys], reads=[B_yst[ys]], writes=[B_y])
                            pending.append((n + 10, finish))
                while pending:
                    pending.pop(0)[1]()
                for g in gens:
                    if g is not conv_g[0]:
                        for _ in g:
                            pass

            def sb_head(hi, gens):
                kind, h = heads[hi]
                b = hi % 2
                units = [(c, i) for c in range(NCH) for i in range(4 * c + 3, -1, -1)]
                N = len(units)
                info = {}
                rcnt = [0]

                def S1(n):
                    c, i = units[n]
                    bk = n % 4
                    diag = i >= 4 * c
                    first = (i == 4 * c + 3)
                    lastu = (i == 0)
                    lo = 128 * (i - 4 * c) if diag else 0
                    info[n] = dict(c=c, i=i, bk=bk, first=first, last=lastu, e=n % 3, L=n % 4, w=n % 4, lo=lo)

                    def s1(e):
                        inst = e.matmul(pb[bk][:, lo:512], lhsT=KA[b][:, i * 128:(i + 1) * 128],
                                        rhs=QA[b][:, c * 512 + lo:(c + 1) * 512], start=True, stop=not diag)
                        if diag:
                            inst = e.matmul(pb[bk][:, lo:512], lhsT=ident[:], rhs=mksb[:, 384:384 + 512 - lo],
                                            start=False, stop=True)
                        return inst
                    P.op("tensor", s1, reads=[B_KA[b], B_QAq[b], B_QAs[b], B_const, B_cb], writes=[PB[bk]])

                def A1(n):
                    d = info[n]
                    lo = d["lo"]
                    P.op("scalar", lambda e: e.activation(out=e32[d["e"]][:, lo:512], in_=pb[d["bk"]][:, lo:512], func=AF.Exp),
                         reads=[PB[d["bk"]]], writes=[B_e32[d["e"]]])

                def A2(n):
                    d = info[n]
                    lo = d["lo"]
                    P.op("scalar", lambda e: e.activation(out=Lb[d["L"]][:, lo:512], in_=e32[d["e"]][:, lo:512], func=AF.Ln,
                                                          bias=1.0, scale=1.0),
                         reads=[B_e32[d["e"]]], writes=[B_Lb[d["L"]]])

                def S3(n):
                    d = info[n]
                    if d["last"]:
                        return
                    lo = d["lo"]
                    rcnt[0] += 1
                    rn = rcnt[0] % 4
                    d["rn"] = rn
                    if d["first"]:
                        P.op("gpsimd", lambda e: e.tensor_copy(out=Rb[rn][:, lo:512], in_=Lb[d["L"]][:, lo:512]),
                             reads=[B_Lb[d["L"]]], writes=[B_Rb[rn]])
                    else:
                        rp = info[n - 1]["rn"]
                        lop = info[n - 1]["lo"]
                        if lop > lo:
                            P.op("gpsimd", lambda e: e.tensor_copy(out=Rb[rn][:, lo:lop], in_=Lb[d["L"]][:, lo:lop]),
                                 reads=[B_Lb[d["L"]]], writes=[B_Rb[rn]])
                        P.op("gpsimd", lambda e: e.tensor_tensor(out=Rb[rn][:, lop:512], in0=Rb[rp][:, lop:512],
                                                                 in1=Lb[d["L"]][:, lop:512], op=ALU.add),
                             reads=[B_Lb[d["L"]], B_Rb[rp]], writes=[B_Rb[rn]])

                def S4(n):
                    d = info[n]
                    lo = d["lo"]
                    rds = [B_Lb[d["L"]], B_const]
                    rp = None
                    lop = 0
                    if not d["first"]:
                        rp = info[n - 1]["rn"]
                        lop = info[n - 1]["lo"]
                        rds.append(B_Rb[rp])

                    def s4(e):
                        inst = e.matmul(pb[d["bk"]][:, lo:512], lhsT=negtri[:], rhs=Lb[d["L"]][:, lo:512], start=False,
                                        stop=d["first"], skip_group_check=True)
                        if rp is not None:
                            inst = e.matmul(pb[d["bk"]][:, lop:512], lhsT=negones[:], rhs=Rb[rp][:, lop:512], start=False,
                                            stop=True, skip_group_check=True)
                        return inst
                    P.op("tensor", s4, reads=rds, writes=[PB[d["bk"]]])

                def A3(n):
                    d = info[n]
                    lo = d["lo"]
                    P.op("scalar", lambda e: e.activation(out=wb[d["w"]][:, lo:512], in_=pb[d["bk"]][:, lo:512], func=AF.Exp),
                         reads=[PB[d["bk"]]], writes=[B_wb[d["w"]]])

                def S6(n):
                    d = info[n]
                    c, i = d["c"], d["i"]
                    lo = d["lo"]
                    yb = 4 + (c % 2)
                    P.op("tensor", lambda e: e.matmul(pb[yb][:, lo:512], lhsT=VAf[b][:, i * 65:i * 65 + 128],
                                                      rhs=wb[d["w"]][:, lo:512],
                                                      start=d["first"], stop=d["last"], skip_group_check=True),
                         reads=[B_VA[b], B_wb[d["w"]]], writes=[PB[yb]])
                    if d["last"]:
                        ys = ycnt[0] % 2
                        ycnt[0] += 1
                        P.op("vector", lambda e: e.tensor_copy(out=yst[ys][:, :], in_=pb[yb][0:64, :]),
                             reads=[PB[yb]], writes=[B_yst[ys]])
                        P.dma(lambda e: e.dma_start(
                            out=y_s[512 + h * 64:512 + (h + 1) * 64, c * 512:(c + 1) * 512], in_=yst[ys][:, :]),
                            B_yst[ys], reads=[B_yst[ys]], writes=[B_y])

                for n in range(N + 3):
                    if n % 5 == 0:
                        for g in gens:
                            next(g, None)
                    if n < N:
                        S1(n)
                        A1(n)
                    if 0 <= n - 1 < N:
                        S4(n - 1)
                    if 0 <= n - 2 < N:
                        A3(n - 2)
                    if 0 <= n - 3 < N:
                        S6(n - 3)
                    if n < N:
                        A2(n)
                        S3(n)
                for g in gens:
                    if g is not conv_g[0]:
                        for _ in g:
                            pass

            conv_g = [conv_gen()]
            load_head(0)
            for hi in range(len(heads)):
                gens = []
                if hi + 1 < len(heads):
                    load_head(hi + 1)
                    if heads[hi + 1][0] == "moba":
                        gens.append(selection(hi + 1))
                if heads[hi][0] == "moba":
                    moba_head(hi, gens)
                else:
                    gens.append(conv_g[0])
                    sb_head(hi, gens)
            for _ in conv_g[0]:
                pass
            P.barrier()
            P.emit()

        with ExitStack() as sc:
            wup = [sbt(sc, f"wup{i}", [128, 8, 256], BF16) for i in range(3)]
            B_wupb = [Buf(f"wup{i}") for i in range(3)]
            wdn = [sbt(sc, f"wdn{i}", [128, D], BF16) for i in range(4)]
            B_wdnb = [Buf(f"wdn{i}") for i in range(4)]
            YT = [sbt(sc, f"YT{i}", [128, 8, 512], BF16) for i in range(2)]
            B_YT = [Buf(f"YT{i}") for i in range(2)]
            G = [sbt(sc, f"G{i}", [128, 2, 512], BF16) for i in range(4)]
            B_G = [Buf(f"G{i}") for i in range(4)]
            xt = [sbt(sc, f"xc{i}", [128, D], F32) for i in range(8)]
            B_xt = [Buf(f"xc{i}") for i in range(8)]
            t1 = [sbt(sc, f"t1_{i}", [128, 512], F32) for i in range(2)]
            t2 = [sbt(sc, f"t2_{i}", [128, 512], F32) for i in range(2)]
            B_t1 = [Buf(f"t1_{i}") for i in range(2)]
            B_t2 = [Buf(f"t2_{i}") for i in range(2)]
            mixT = [sbt(sc, f"mixT{m}", [128, 8, 512], BF16) for m in range(2)]
            B_mixT = [[Buf(f"mixT{m}_{i}") for i in range(8)] for m in range(2)]
            junk = sbt(sc, "junkc", [128, D], BF16)
            B_junk = Buf("junkc")
            ss = sbt(sc, "ssc", [128, 4], F32)
            ms = sbt(sc, "msc", [128, 4], F32)
            rstd = sbt(sc, "rstdc", [128, 4], F32)
            B_ss, B_ms, B_rstd = Buf("ssc"), Buf("msc"), Buf("rstdc")
            ss2 = sbt(sc, "ss2", [128, 4], F32)
            ms2 = sbt(sc, "ms2", [128, 4], F32)
            rstd2 = sbt(sc, "rstd2", [128, 4], F32)
            B_ss2, B_ms2, B_rstd2 = Buf("ss2"), Buf("ms2"), Buf("rstd2")
            hn = [sbt(sc, f"hnc{i}", [128, D], BF16) for i in range(4)]
            B_hn = [Buf(f"hnc{i}") for i in range(4)]
            h2T = sbt(sc, "h2T", [128, 8, 512], BF16)
            B_h2T = Buf("h2T")
            mT = sbt(sc, "mT", [128, NJ, 512], BF16)
            B_mT = [Buf(f"mT{i}") for i in range(NJ)]
            raw = [sbt(sc, f"raw{i}", [128, 514], F32) for i in range(3)]
            B_raw = [Buf(f"raw{i}") for i in range(3)]
            tt = [sbt(sc, f"tt{i}", [128, 512], F32) for i in range(3)]
            B_tt = [Buf(f"tt{i}") for i in range(3)]
            uu = [sbt(sc, f"uu{i}", [128, 512], F32) for i in range(4)]
            B_uu = [Buf(f"uu{i}") for i in range(4)]
            sg = [sbt(sc, f"sg{i}", [128, 512], F32) for i in range(2)]
            B_sg = [Buf(f"sg{i}") for i in range(2)]
            pbTs = [pb[6][:].bitcast(BF16), pb[7][:].bitcast(BF16)]

            def load_x(c):
                for s in range(4):
                    xi = (c % 2) * 4 + s
                    t0 = c * 512 + s * 128
                    P.dma(lambda e, xi=xi, t0=t0: e.dma_start(out=xt[xi][:], in_=x_d[t0:t0 + 128, :]),
                          B_xt[xi], writes=[B_xt[xi]])

            def load_YT(c):
                yb_ = c % 2
                t0c = c * 512
                P.dma(lambda e: e.dma_start(
                    out=YT[yb_][:], in_=y_s[:, t0c:t0c + 512].rearrange("(k p) t -> p k t", p=128)),
                    B_YT[yb_], reads=[B_y], writes=[B_YT[yb_]])

            g_iss = [0]

            def ensure_G(upto):
                while g_iss[0] <= min(upto, NCH * 8 - 1):
                    idx = g_iss[0]
                    g_iss[0] += 1
                    c_, f_ = idx // 8, idx % 8
                    gb = idx % 4
                    t0c = c_ * 512
                    P.dma(lambda e, gb=gb, f_=f_, t0c=t0c: e.dma_start(
                        out=G[gb][:], in_=g_s[:, t0c:t0c + 512].rearrange("(a r) t -> r a t", a=2)[f_ * 128:(f_ + 1) * 128]),
                        B_G[gb], reads=[B_g], writes=[B_G[gb]])

            u_iss = [0]

            def ensure_wup(upto):
                while u_iss[0] <= min(upto, NCH * NJ - 1):
                    idx = u_iss[0]
                    u_iss[0] += 1
                    j_ = idx % NJ
                    wb_ = idx % 3
                    P.dma(lambda e, wb_=wb_, j_=j_: e.dma_start(out=wup[wb_][:, :, 0:128], in_=wup_s[j_]),
                          B_wupb[wb_], reads=[B_wup], writes=[B_wupb[wb_]])
                    P.dma(lambda e, wb_=wb_, j_=j_: e.dma_start(out=wup[wb_][:, :, 128:256], in_=wup_s[NJ + j_]),
                          B_wupb[wb_], reads=[B_wup], writes=[B_wupb[wb_]])

            d_iss = [0]

            def ensure_wdn(upto):
                while d_iss[0] <= min(upto, NCH * 2 * NJ - 1):
                    idx = d_iss[0]
                    d_iss[0] += 1
                    j_ = idx % NJ
                    db = idx % 4
                    P.dma(lambda e, db=db, j_=j_: e.dma_start(out=wdn[db][:], in_=wdn_s[j_ * 128:(j_ + 1) * 128, :]),
                          B_wdnb[db], reads=[B_wdn], writes=[B_wdnb[db]])

            load_x(0)
            load_YT(0)
            ensure_G(1)
            bki = [0]

            def nextbank():
                bk = bki[0] % 6
                bki[0] += 1
                return bk

            ri = [0]
            oi = [0]
            prev_silu = [None]

            def P1(c):
                t0c = c * 512
                ycb = c % 2
                mb = c % 2
                for f in range(8):
                    gb = (c * 8 + f) % 4
                    ensure_G(c * 8 + f + 2)
                    bka = nextbank()
                    bkb = nextbank()

                    def mma(e, f=f, bka=bka, ycb=ycb):
                        inst = None
                        for k in range(4):
                            inst = e.matmul(pb[bka][:], lhsT=wpm[:, k, f * 128:(f + 1) * 128], rhs=YT[ycb][:, k, :],
                                            start=(k == 0), stop=(k == 3))
                        return inst

                    def mmb(e, f=f, bkb=bkb, ycb=ycb):
                        inst = None
                        for k in range(4):
                            inst = e.matmul(pb[bkb][:], lhsT=wps[:, k, f * 128:(f + 1) * 128], rhs=YT[ycb][:, 4 + k, :],
                                            start=(k == 0), stop=(k == 3))
                        return inst
                    P.op("tensor", mma, reads=[B_wres, B_YT[ycb]], writes=[PB[bka]])
                    P.op("tensor", mmb, reads=[B_wres, B_YT[ycb]], writes=[PB[bkb]])
                    tb = f % 2
                    P.op("vector", lambda e, bka=bka, gb=gb, tb=tb: e.tensor_tensor(
                        out=t1[tb][:], in0=pb[bka][:], in1=G[gb][:, 0, :], op=ALU.mult),
                        reads=[PB[bka], B_G[gb]], writes=[B_t1[tb]])
                    P.op("vector", lambda e, bkb=bkb, gb=gb, tb=tb: e.tensor_tensor(
                        out=t2[tb][:], in0=pb[bkb][:], in1=G[gb][:, 1, :], op=ALU.mult),
                        reads=[PB[bkb], B_G[gb]], writes=[B_t2[tb]])
                    P.op("gpsimd", lambda e, f=f, tb=tb, mb=mb: e.tensor_tensor(
                        out=mixT[mb][:, f, :], in0=t1[tb][:], in1=t2[tb][:], op=ALU.add),
                        reads=[B_t1[tb], B_t2[tb]], writes=[B_mixT[mb][f]])

            def P2(c):
                mb = c % 2
                for s in range(4):
                    xi = (c % 2) * 4 + s
                    for hf in range(2):
                        bk = nextbank()

                        def mmo(e, s=s, hf=hf, bk=bk, mb=mb):
                            inst = None
                            for f in range(8):
                                inst = e.matmul(pb[bk][:], lhsT=mixT[mb][:, f, s * 128:(s + 1) * 128],
                                                rhs=wout[:, f, hf * 512:(hf + 1) * 512], start=(f == 0), stop=(f == 7))
                            return inst
                        P.op("tensor", mmo, reads=B_mixT[mb] + [B_wres], writes=[PB[bk]])
                        P.op("vector", lambda e, xi=xi, hf=hf, bk=bk: e.tensor_tensor(
                            out=xt[xi][:, hf * 512:(hf + 1) * 512], in0=pb[bk][:], in1=xt[xi][:, hf * 512:(hf + 1) * 512],
                            op=ALU.add),
                            reads=[PB[bk]], writes=[B_xt[xi]])
                    P.op("scalar", lambda e, xi=xi, s=s: e.activation(
                        out=junk[:], in_=xt[xi][:], func=AF.Square, accum_out=ss[:, s:s + 1]),
                        reads=[B_xt[xi]], writes=[B_junk, B_ss])

            def P3a(c):
                rstd_from_ss(ss, ms, rstd, B_ss, B_ms, B_rstd, 4)
                for s in range(4):
                    xi = (c % 2) * 4 + s
                    hb = s
                    P.op("vector", lambda e, xi=xi, s=s, hb=hb: e.tensor_scalar(
                        out=hn[hb][:], in0=xt[xi][:], scalar1=rstd[:, s:s + 1], scalar2=None, op0=ALU.mult),
                        reads=[B_xt[xi], B_rstd], writes=[B_hn[hb]])

            def P3b(c):
                for s in range(4):
                    hb = s
                    pbi = s % 2

                    def tr(e, hb=hb, pbi=pbi):
                        inst = None
                        for j in range(8):
                            inst = e.transpose(out=pbTs[pbi][:, j * 128:(j + 1) * 128], in_=hn[hb][:, j * 128:(j + 1) * 128],
                                               identity=ident[:])
                        return inst
                    P.op("tensor", tr, reads=[B_hn[hb], B_const], writes=[PB[6 + pbi]])
                    P.op("scalar", lambda e, s=s, pbi=pbi: e.activation(
                        out=h2T[:, :, s * 128:(s + 1) * 128], in_=pbTs[pbi].rearrange("p (j t) -> p j t", j=8), func=AF.Copy),
                        reads=[PB[6 + pbi]], writes=[B_h2T])

            def P4(c):
                for j in range(NJ):
                    wb_ = (c * NJ + j) % 3
                    ensure_wup(c * NJ + j + 2)
                    if j == NJ - 3:
                        ensure_wdn(c * 2 * NJ + 3)
                    res = []
                    for ab in range(2):
                        jj = ab * NJ + j
                        bk = nextbank()

                        def mmu(e, wb_=wb_, ab=ab, bk=bk):
                            inst = None
                            for k in range(8):
                                inst = e.matmul(pb[bk][:], lhsT=wup[wb_][:, k, ab * 128:(ab + 1) * 128], rhs=h2T[:, k, :],
                                                start=(k == 0), stop=(k == 7))
                            return inst
                        P.op("tensor", mmu, reads=[B_wupb[wb_], B_h2T], writes=[PB[bk]])
                        rb = ri[0] % 3
                        ub = ri[0] % 4
                        ri[0] += 1
                        P.op("scalar", lambda e, rb=rb, bk=bk: e.activation(out=raw[rb][:, 2:514], in_=pb[bk][:], func=AF.Copy),
                             reads=[PB[bk]], writes=[B_raw[rb]])
                        P.op("scalar", lambda e, rb=rb, bk=bk, jj=jj: e.activation(
                            out=tt[rb][:], in_=pb[bk][:], func=AF.Identity, scale=cw[:, jj, 2:3], bias=cbt[:, jj:jj + 1]),
                            reads=[PB[bk], B_cc], writes=[B_tt[rb]])
                        P.op("gpsimd", lambda e, rb=rb, jj=jj: e.tensor_copy(out=raw[rb][:, 0:2], in_=halo[:, jj, :]),
                             reads=[B_halo], writes=[B_raw[rb]])
                        P.op("gpsimd", lambda e, rb=rb, jj=jj: e.tensor_copy(out=halo[:, jj, :], in_=raw[rb][:, 512:514]),
                             reads=[B_raw[rb]], writes=[B_halo])
                        P.op("vector", lambda e, rb=rb, jj=jj: e.scalar_tensor_tensor(
                            out=tt[rb][:], in0=raw[rb][:, 1:513], scalar=cw[:, jj, 1:2], in1=tt[rb][:],
                            op0=ALU.mult, op1=ALU.add),
                            reads=[B_raw[rb], B_cc], writes=[B_tt[rb]])
                        P.op("vector", lambda e, rb=rb, jj=jj, ub=ub: e.scalar_tensor_tensor(
                            out=uu[ub][:], in0=raw[rb][:, 0:512], scalar=cw[:, jj, 0:1], in1=tt[rb][:],
                            op0=ALU.mult, op1=ALU.add),
                            reads=[B_raw[rb], B_tt[rb], B_cc], writes=[B_uu[ub]])
                        res.append(ub)
                    if prev_silu[0] is not None:
                        prev_silu[0]()

                    def do_silu(j=j, res=tuple(res)):
                        sgb = j % 2
                        P.op("scalar", lambda e: e.activation(out=sg[sgb][:], in_=uu[res[0]][:], func=AF.Silu),
                             reads=[B_uu[res[0]]], writes=[B_sg[sgb]])
                        P.op("gpsimd", lambda e: e.tensor_tensor(
                            out=mT[:, j, :], in0=sg[sgb][:], in1=uu[res[1]][:], op=ALU.mult),
                            reads=[B_sg[sgb], B_uu[res[1]]], writes=[B_mT[j]])
                    prev_silu[0] = do_silu
                prev_silu[0]()
                prev_silu[0] = None

            def P5(c):
                for sp2 in range(2):
                    bks = [[nextbank() for hf in range(2)] for s in range(2)]
                    for j in range(NJ):
                        didx = (c * 2 + sp2) * NJ + j
                        db = didx % 4
                        ensure_wdn(didx + 3)

                        def mmd(e, j=j, db=db, bks=bks, sp2=sp2):
                            inst = None
                            for s in range(2):
                                for hf in range(2):
                                    tok = (sp2 * 2 + s) * 128
                                    inst = e.matmul(pb[bks[s][hf]][:], lhsT=mT[:, j, tok:tok + 128],
                                                    rhs=wdn[db][:, hf * 512:(hf + 1) * 512], start=(j == 0),
                                                    stop=(j == NJ - 1))
                            return inst
                        P.op("tensor", mmd, reads=[B_mT[j], B_wdnb[db]], writes=[PB[bks[s][hf]] for s in range(2) for hf in range(2)])
                    for s in range(2):
                        sidx = sp2 * 2 + s
                        xi = (c % 2) * 4 + sidx
                        for hf in range(2):
                            bk = bks[s][hf]
                            P.op("vector", lambda e, xi=xi, hf=hf, bk=bk: e.tensor_tensor(
                                out=xt[xi][:, hf * 512:(hf + 1) * 512], in0=pb[bk][:], in1=xt[xi][:, hf * 512:(hf + 1) * 512],
                                op=ALU.add),
                                reads=[PB[bk]], writes=[B_xt[xi]])
                        P.op("scalar", lambda e, xi=xi, sidx=sidx: e.activation(
                            out=junk[:], in_=xt[xi][:], func=AF.Square, accum_out=ss2[:, sidx:sidx + 1]),
                            reads=[B_xt[xi]], writes=[B_junk, B_ss2])

            def F1(c):
                rstd_from_ss(ss2, ms2, rstd2, B_ss2, B_ms2, B_rstd2, 4)

            def F2(c):
                for s in range(4):
                    xi = (c % 2) * 4 + s
                    P.op("vector", lambda e, xi=xi, s=s: e.scalar_tensor_tensor(
                        out=xt[xi][:], in0=xt[xi][:], scalar=rstd2[:, s:s + 1], in1=gfin[:], op0=ALU.mult, op1=ALU.mult),
                        reads=[B_rstd2, B_cc], writes=[B_xt[xi]])
                    tok = c * 512 + s * 128
                    P.dma(lambda e, xi=xi, tok=tok: e.dma_start(out=out_d[tok:tok + 128, :], in_=xt[xi][:]),
                          B_xt[xi], reads=[B_xt[xi]], writes=[B_out], eng="scalar")

            if NCH > 1:
                load_YT(1)
            P1(0)
            for c in range(NCH):
                ensure_wup(c * NJ + 1)
                P2(c)
                if c > 0:
                    F2(c - 1)
                if c + 1 < NCH:
                    load_x(c + 1)
                P3a(c)
                if c + 1 < NCH:
                    P1(c + 1)
                P3b(c)
                if c + 2 < NCH:
                    load_YT(c + 2)
                    ensure_G((c + 2) * 8 + 1)
                P4(c)
                P5(c)
                F1(c)
            F2(NCH - 1)
            P.barrier()
            P.emit()
    return nc


_CACHE = {}


def _prep_shared(w_in, w_proj_moba, w_proj_sb, w_out, w_up, w_down, g_mix, g_ffn, g_final, conv_w, conv_b, T):
    f = np.float32
    d = {}
    d["w_in"] = np.ascontiguousarray(np.asarray(w_in, f)[0])
    d["w_pm"] = np.ascontiguousarray(np.asarray(w_proj_moba, f)[0])
    d["w_ps"] = np.ascontiguousarray(np.asarray(w_proj_sb, f)[0])
    d["w_out"] = np.ascontiguousarray(np.asarray(w_out, f)[0])
    d["w_up"] = np.ascontiguousarray(np.asarray(w_up, f)[0])
    d["w_down"] = np.ascontiguousarray(np.asarray(w_down, f)[0])
    d["g_mix"] = np.ascontiguousarray(np.asarray(g_mix, f)[0].reshape(8, 128).T)
    d["g_ffn"] = np.ascontiguousarray(np.asarray(g_ffn, f)[0].reshape(8, 128).T)
    d["g_fin"] = np.ascontiguousarray(np.broadcast_to(np.asarray(g_final, f).reshape(1, D), (128, D)))
    cw = np.asarray(conv_w, f)[0, :, 0, :]
    d["cw"] = np.ascontiguousarray(cw.reshape(3, 2 * NJ, 128).transpose(2, 1, 0))
    d["cb"] = np.ascontiguousarray(np.asarray(conv_b, f)[0].reshape(2 * NJ, 128).T)
    d.update(make_consts(T))
    return d


def kernel(x, g_mix, w_in, w_proj_moba, w_proj_sb, w_out, g_ffn, w_up, conv_w, conv_b, w_down, g_final):
    x = np.asarray(x, np.float32)
    B, T, _ = x.shape
    if T not in _CACHE:
        _CACHE[T] = build(T)
    nc = _CACHE[T]
    shared = _prep_shared(w_in, w_proj_moba, w_proj_sb, w_out, w_up, w_down, g_mix, g_ffn, g_final, conv_w, conv_b, T)
    in_maps = []
    for b in range(B):
        m = dict(shared)
        m["x"] = np.ascontiguousarray(x[b])
        in_maps.append(m)
    res = run_bass_kernel_spmd(nc, in_maps, core_ids=list(range(B)))
    out = np.stack([np.asarray(r["out"], np.float32) for r in res.results], axis=0)
    return out
```

```python
import numpy as np
import ml_dtypes
from contextlib import ExitStack
import concourse.bass as bass
import concourse.mybir as mybir
from concourse.bass_utils import run_bass_kernel_spmd

F32 = mybir.dt.float32
BF16 = mybir.dt.bfloat16
AF = mybir.ActivationFunctionType
ALU = mybir.AluOpType
AX = mybir.AxisListType

D = 1024
DIN = 5120
DFF = 2816
NJ = DFF // 128
NH = 8
DH = 64
BIG = 32768.0
EPS = 1e-6
ENGS = ("sync", "scalar", "vector", "gpsimd", "tensor")


class Buf:
    __slots__ = ("name", "w", "r", "semkey")

    def __init__(self, name):
        self.name = name
        self.w = {}
        self.r = {}
        self.semkey = None


def _merge(d, s):
    for k, c in s.items():
        if d.get(k, 0) < c:
            d[k] = c


class Prog:
    def __init__(self, nc, es):
        self.nc = nc
        self.es = es
        self.sems = {}
        self.cnt = {}
        self.ops = {e: [] for e in ENGS}
        self.waited = {e: {} for e in ENGS}
        for e in ENGS:
            self._newsem(e)

    def _newsem(self, key):
        self.sems[key] = self.es.enter_context(self.nc.semaphore("s_" + key))
        self.cnt[key] = 0

    def _deps(self, eng, reads, writes, extra):
        deps = {}
        for b in reads:
            _merge(deps, b.w)
        for b in writes:
            _merge(deps, b.r)
            _merge(deps, b.w)
        for e in extra:
            if e:
                _merge(deps, e)
        waits = []
        wd = self.waited[eng]
        for k, c in deps.items():
            if eng == "tensor" and k == "tensor":
                continue
            if wd.get(k, 0) >= c:
                continue
            wd[k] = c
            waits.append((k, c))
        return waits

    def _update(self, ev, reads, writes):
        for b in writes:
            if b.r:
                b.r = {}
                b.w = {}
            _merge(b.w, ev)
        for b in reads:
            if b not in writes:
                _merge(b.r, ev)

    def op(self, eng, fn, reads=(), writes=(), extra=()):
        waits = self._deps(eng, reads, writes, extra)
        self.cnt[eng] += 1
        ev = {eng: self.cnt[eng]}
        self.ops[eng].append((waits, fn, (eng, 1)))
        self._update(ev, reads, writes)
        return ev

    def dma(self, fn, owner, reads=(), writes=(), extra=(), eng="sync"):
        if owner.semkey is None:
            owner.semkey = "d_" + owner.name
            self._newsem(owner.semkey)
        key = owner.semkey
        waits = self._deps(eng, reads, writes, extra)
        self.cnt[key] += 16
        ev = {key: self.cnt[key]}
        self.ops[eng].append((waits, fn, (key, 16)))
        self._update(ev, reads, writes)
        return ev

    def barrier(self):
        allc = {k: c for k, c in self.cnt.items() if c > 0}
        for eng in ENGS:
            waits = []
            wd = self.waited[eng]
            for k, c in allc.items():
                if k == eng:
                    continue
                if wd.get(k, 0) >= c:
                    continue
                wd[k] = c
                waits.append((k, c))
            if waits:
                self.ops[eng].append((waits, None, None))

    def emit(self):
        nc = self.nc
        sems = self.sems
        ops = self.ops
        self.ops = {e: [] for e in ENGS}
        with nc.Block() as block:
            def runner(name):
                def run(e):
                    for waits, fn, inc in ops[name]:
                        for k, c in waits:
                            e.wait_ge(sems[k], c)
                        if fn is not None:
                            inst = fn(e)
                            inst.then_inc(sems[inc[0]], inc[1])
                return run
            block.sync(runner("sync"))
            block.scalar(runner("scalar"))
            block.vector(runner("vector"))
            block.gpsimd(runner("gpsimd"))
            block.tensor(runner("tensor"))


def make_consts(T):
    bf = ml_dtypes.bfloat16
    NT = T // 128
    nblk = T // 256
    c = {}
    c["c_ident"] = np.eye(128, dtype=np.float32).astype(bf)
    j = np.arange(128)[:, None]
    k = np.arange(128)[None, :]
    c["c_negtri"] = np.where(j >= k, -1.0, 0.0).astype(bf)
    c["c_negones"] = np.full((128, 128), -1.0, np.float32).astype(bf)
    allneg = np.full((128, 128), -BIG, np.float32)
    zero = np.zeros((128, 128), np.float32)
    strict = np.where(j >= k, -BIG, 0.0).astype(np.float32)
    incl = np.where(j > k, -BIG, 0.0).astype(np.float32)
    c["c_mksb"] = np.concatenate([allneg] * 3 + [strict] + [zero] * 3, axis=1).astype(bf)
    c["c_mkmo"] = np.concatenate([allneg] * 3 + [incl] + [zero] * 3, axis=1).astype(bf)
    pos = np.arange(T)
    ind = (pos[None, :] // 256 == np.arange(nblk)[:, None]).astype(np.float32)
    c["c_ind"] = ind.astype(bf)
    slopes = 2.0 ** (-8.0 * np.arange(1, NH + 1) / NH)
    qal = np.zeros((NH, 4, T), np.float32)
    kal = np.zeros((NH, 4, T), np.float32)
    for h in range(NH):
        s = slopes[h]
        qal[h, 0] = -s * (pos % 128)
        qal[h, 1] = -s * 128.0 * (pos // 128)
        qal[h, 2] = 1.0
        qal[h, 3] = 1.0
        kal[h, 0] = 1.0
        kal[h, 1] = 1.0
        kal[h, 2] = s * (pos % 128)
        kal[h, 3] = s * 128.0 * (pos // 128)
    c["c_qal"] = qal.astype(bf)
    c["c_kal"] = kal.astype(bf)
    qt = np.arange(NT)[:, None]
    n = np.arange(nblk)[None, :]
    own = qt // 2
    pm = np.where(n < own, 0.0, -BIG).astype(np.float32)
    um = np.where(n < own, 0.0, np.where(n == own, BIG, -BIG)).astype(np.float32)
    c["c_pm"] = np.ascontiguousarray(np.broadcast_to(pm.reshape(1, -1), (128, NT * nblk))).astype(bf)
    c["c_um"] = np.ascontiguousarray(np.broadcast_to(um.reshape(1, -1), (128, NT * nblk))).astype(bf)
    return c


def build(T):
    NT = T // 128
    NCH = T // 512
    nblk = T // 256
    assert T % 512 == 0 and 8 <= nblk <= 32
    KR = 100

    nc = bass.Bass("TRN2", target_bir_lowering=False)

    def din(name, shape, dt=F32):
        return nc.dram_tensor(name, list(shape), dt, kind="ExternalInput").ap()

    x_d = din("x", [T, D])
    win_d = din("w_in", [D, DIN])
    wpm_d = din("w_pm", [512, D])
    wps_d = din("w_ps", [512, D])
    wout_d = din("w_out", [D, D])
    wup_d = din("w_up", [D, 2 * DFF])
    wdn_d = din("w_down", [DFF, D])
    gmix_d = din("g_mix", [128, 8])
    gffn_d = din("g_ffn", [128, 8])
    gfin_d = din("g_fin", [128, D])
    cw_d = din("cw", [128, 2 * NJ, 3])
    cb_d = din("cb", [128, 2 * NJ])
    ident_d = din("c_ident", [128, 128], BF16)
    negtri_d = din("c_negtri", [128, 128], BF16)
    negones_d = din("c_negones", [128, 128], BF16)
    mksb_d = din("c_mksb", [128, 896], BF16)
    mkmo_d = din("c_mkmo", [128, 896], BF16)
    ind_d = din("c_ind", [nblk, T], BF16)
    qal_d = din("c_qal", [NH, 4, T], BF16)
    kal_d = din("c_kal", [NH, 4, T], BF16)
    pm_d = din("c_pm", [128, NT * nblk], BF16)
    um_d = din("c_um", [128, NT * nblk], BF16)
    out_d = nc.dram_tensor("out", [T, D], F32, kind="ExternalOutput").ap()

    qt_s = nc.dram_tensor("qt_s", [16 * 64, T], BF16).ap()
    kt_s = nc.dram_tensor("kt_s", [16 * 64, T], BF16).ap()
    v_s = nc.dram_tensor("v_s", [16, T, 64], BF16).ap()
    g_s = nc.dram_tensor("g_s", [2 * D, T], BF16).ap()
    y_s = nc.dram_tensor("y_s", [D, T], BF16).ap()
    wup_s = nc.dram_tensor("wup_s", [2 * NJ, 128, 8, 128], BF16).ap()
    wdn_s = nc.dram_tensor("wdn_s", [DFF, D], BF16).ap()
    B_qt, B_kt, B_v, B_g, B_y, B_wup, B_wdn, B_out = (Buf(n) for n in
                                                     ("qt_s", "kt_s", "v_s", "g_s", "y_s", "wup_s", "wdn_s", "out"))

    with ExitStack() as es:
        P = Prog(nc, es)

        def sbt(st, name, shape, dt):
            return st.enter_context(nc.sbuf_tensor("sb_" + name, list(shape), dt))

        pb = [es.enter_context(nc.psum_tensor(f"pb{i}", [128, 512], F32)) for i in range(8)]
        PB = [Buf(f"pb{i}") for i in range(8)]

        ident = sbt(es, "ident", [128, 128], BF16)
        negtri = sbt(es, "negtri", [128, 128], BF16)
        negones = sbt(es, "negones", [128, 128], BF16)
        negh = sbt(es, "negh", [128, 4], F32)
        ones32 = sbt(es, "ones32", [128, 128], F32)
        B_const = Buf("const")

        P.dma(lambda e: e.dma_start(out=ident[:], in_=ident_d), B_const, writes=[B_const])
        P.dma(lambda e: e.dma_start(out=negtri[:], in_=negtri_d), B_const, writes=[B_const])
        P.dma(lambda e: e.dma_start(out=negones[:], in_=negones_d), B_const, writes=[B_const])
        P.op("vector", lambda e: e.memset(negh[:], -0.5), writes=[B_const])
        P.op("vector", lambda e: e.memset(ones32[:], 1.0), writes=[B_const])

        def rstd_from_ss(ss, ms, rstd, B_ss, B_ms, B_rstd, n):
            P.op("vector", lambda e: e.tensor_scalar(out=ms[:, 0:n], in0=ss[:, 0:n], scalar1=1.0 / D, scalar2=EPS,
                                                     op0=ALU.mult, op1=ALU.add),
                 reads=[B_ss], writes=[B_ms])
            P.op("gpsimd", lambda e: e.tensor_tensor(out=rstd[:, 0:n], in0=ms[:, 0:n], in1=negh[:, 0:n], op=ALU.pow),
                 reads=[B_ms, B_const], writes=[B_rstd])

        with ExitStack() as sa:
            winb = sbt(sa, "winb", [128, 8, DIN], BF16)
            wst = [sbt(sa, f"wst{i}", [128, 2560], F32) for i in range(2)]
            B_wst = [Buf(f"wst{i}") for i in range(2)]
            B_winb = Buf("winb")
            gmix = sbt(sa, "gmix", [128, 8], F32)
            B_gmix = Buf("gmix")
            xt = [sbt(sa, f"xt{i}", [128, D], F32) for i in range(8)]
            B_xt = [Buf(f"xt{i}") for i in range(8)]
            junk = sbt(sa, "junk", [128, D], BF16)
            B_junk = Buf("junk")
            ss = [sbt(sa, f"ss{i}", [128, 4], F32) for i in range(2)]
            ms = [sbt(sa, f"ms{i}", [128, 4], F32) for i in range(2)]
            rstd = [sbt(sa, f"rstd{i}", [128, 4], F32) for i in range(2)]
            B_ss = [Buf(f"ss{i}") for i in range(2)]
            B_ms = [Buf(f"ms{i}") for i in range(2)]
            B_rstd = [Buf(f"rstd{i}") for i in range(2)]
            hn = [sbt(sa, f"hn{i}", [128, D], BF16) for i in range(2)]
            B_hn = [Buf(f"hn{i}") for i in range(2)]
            hT = [sbt(sa, f"hT{i}", [128, 8, 512], BF16) for i in range(2)]
            B_hT = [Buf(f"hT{i}") for i in range(2)]
            stg = [sbt(sa, f"stg{i}", [128, 4, 512], BF16) for i in range(4)]
            B_stg = [Buf(f"stg{i}") for i in range(4)]

            P.dma(lambda e: e.dma_start(out=gmix[:], in_=gmix_d), B_gmix, writes=[B_gmix])
            pi = 0
            B_winh = [Buf("winb0"), Buf("winb1")]
            for hf in range(2):
                for j in range(8):
                    b = pi % 2
                    P.dma(lambda e, j=j, hf=hf, b=b: e.dma_start(
                        out=wst[b][:], in_=win_d[j * 128:(j + 1) * 128, hf * 2560:(hf + 1) * 2560]),
                        B_wst[b], writes=[B_wst[b]])
                    if pi % 2 == 0:
                        P.op("vector", lambda e, j=j, hf=hf, b=b: e.tensor_scalar(
                            out=winb[:, j, hf * 2560:(hf + 1) * 2560], in0=wst[b][:], scalar1=gmix[:, j:j + 1],
                            scalar2=None, op0=ALU.mult),
                            reads=[B_wst[b], B_gmix], writes=[B_winh[hf]])
                    else:
                        P.op("scalar", lambda e, j=j, hf=hf, b=b: e.activation(
                            out=winb[:, j, hf * 2560:(hf + 1) * 2560], in_=wst[b][:], func=AF.Copy, scale=gmix[:, j:j + 1]),
                            reads=[B_wst[b], B_gmix], writes=[B_winh[hf]])
                    pi += 1

            pbTa = [pb[6][:].bitcast(BF16), pb[7][:].bitcast(BF16)]

            def load_x(c):
                for s in range(4):
                    xi = (c % 2) * 4 + s
                    t0 = c * 512 + s * 128
                    P.dma(lambda e, xi=xi, t0=t0: e.dma_start(out=xt[xi][:], in_=x_d[t0:t0 + 128, :]),
                          B_xt[xi], writes=[B_xt[xi]])

            load_x(0)
            stg_i = 0
            evac_i = 0
            for c in range(NCH):
                cb_ = c % 2
                if c + 1 < NCH:
                    load_x(c + 1)
                for s in range(4):
                    xi = cb_ * 4 + s
                    P.op("scalar", lambda e, xi=xi, s=s, cb_=cb_: e.activation(
                        out=junk[:], in_=xt[xi][:], func=AF.Square, accum_out=ss[cb_][:, s:s + 1]),
                        reads=[B_xt[xi]], writes=[B_junk, B_ss[cb_]])
                rstd_from_ss(ss[cb_], ms[cb_], rstd[cb_], B_ss[cb_], B_ms[cb_], B_rstd[cb_], 4)
                for s in range(4):
                    xi = cb_ * 4 + s
                    hb = s % 2
                    P.op("vector", lambda e, xi=xi, s=s, hb=hb, cb_=cb_: e.tensor_scalar(
                        out=hn[hb][:], in0=xt[xi][:], scalar1=rstd[cb_][:, s:s + 1], scalar2=None, op0=ALU.mult),
                        reads=[B_xt[xi], B_rstd[cb_]], writes=[B_hn[hb]])

                    def tr(e, hb=hb):
                        inst = None
                        for j in range(8):
                            inst = e.transpose(out=pbTa[hb][:, j * 128:(j + 1) * 128], in_=hn[hb][:, j * 128:(j + 1) * 128],
                                               identity=ident[:])
                        return inst
                    P.op("tensor", tr, reads=[B_hn[hb], B_const], writes=[PB[6 + hb]])
                    P.op("vector", lambda e, s=s, cb_=cb_, hb=hb: e.tensor_copy(
                        out=hT[cb_][:, :, s * 128:(s + 1) * 128],
                        in_=pbTa[hb].rearrange("p (j t) -> p j t", j=8)),
                        reads=[PB[6 + hb]], writes=[B_hT[cb_]])

                for grp in range(10):
                    col0 = grp * 512
                    if grp in (2, 5):
                        sb_ = stg_i % 4
                        stg_i += 1
                        for s in range(4):
                            bk = evac_i % 6
                            evac_i += 1

                            def mmv(e, s=s, bk=bk, col0=col0, cb_=cb_):
                                inst = None
                                for j in range(8):
                                    inst = e.matmul(pb[bk][:], lhsT=hT[cb_][:, j, s * 128:(s + 1) * 128],
                                                    rhs=winb[:, j, col0:col0 + 512], start=(j == 0), stop=(j == 7))
                                return inst
                            P.op("tensor", mmv, reads=[B_hT[cb_], B_winh[grp // 5]], writes=[PB[bk]])
                            P.op("vector", lambda e, s=s, bk=bk, sb_=sb_: e.tensor_copy(out=stg[sb_][:, s, :], in_=pb[bk][:]),
                                 reads=[PB[bk]], writes=[B_stg[sb_]])
                        hg0 = 0 if grp == 2 else 8
                        for s in range(4):
                            t0 = c * 512 + s * 128
                            P.dma(lambda e, s=s, sb_=sb_, hg0=hg0, t0=t0: e.dma_start(
                                out=v_s[hg0:hg0 + 8, t0:t0 + 128, :].rearrange("h t d -> t h d"),
                                in_=stg[sb_][:, s, :].rearrange("p (h d) -> p h d", h=8)),
                                B_stg[sb_], reads=[B_stg[sb_]], writes=[B_v])
                    else:
                        sb_ = stg_i % 4
                        stg_i += 1
                        for q in range(4):
                            bk = evac_i % 6
                            evac_i += 1
                            cc = col0 + q * 128

                            def mmf(e, bk=bk, cc=cc, cb_=cb_):
                                inst = None
                                for j in range(8):
                                    inst = e.matmul(pb[bk][:], lhsT=winb[:, j, cc:cc + 128], rhs=hT[cb_][:, j, :],
                                                    start=(j == 0), stop=(j == 7))
                                return inst
                            P.op("tensor", mmf, reads=[B_hT[cb_], B_winh[grp // 5]], writes=[PB[bk]])
                            if grp >= 6:
                                P.op("scalar", lambda e, bk=bk, sb_=sb_, q=q: e.activation(
                                    out=stg[sb_][:, q, :], in_=pb[bk][:], func=AF.Sigmoid),
                                    reads=[PB[bk]], writes=[B_stg[sb_]])
                            elif grp in (0, 3):
                                P.op("scalar", lambda e, bk=bk, sb_=sb_, q=q: e.activation(
                                    out=stg[sb_][:, q, :], in_=pb[bk][:], func=AF.Copy, scale=0.125),
                                    reads=[PB[bk]], writes=[B_stg[sb_]])
                            else:
                                P.op("vector", lambda e, bk=bk, sb_=sb_, q=q: e.tensor_copy(
                                    out=stg[sb_][:, q, :], in_=pb[bk][:]),
                                    reads=[PB[bk]], writes=[B_stg[sb_]])
                        t0 = c * 512
                        if grp in (0, 3):
                            dst, r0, Bd = qt_s, (0 if grp == 0 else 512), B_qt
                        elif grp in (1, 4):
                            dst, r0, Bd = kt_s, (0 if grp == 1 else 512), B_kt
                        else:
                            dst, r0, Bd = g_s, (grp - 6) * 512, B_g
                        P.dma(lambda e, dst=dst, r0=r0, t0=t0, sb_=sb_: e.dma_start(
                            out=dst[r0:r0 + 512, t0:t0 + 512].rearrange("(g p) t -> p g t", p=128),
                            in_=stg[sb_][:]),
                            B_stg[sb_], reads=[B_stg[sb_]], writes=[Bd])
            P.barrier()
            P.emit()

        wpm = sbt(es, "wpm", [128, 4, D], BF16)
        wps = sbt(es, "wps", [128, 4, D], BF16)
        wout = sbt(es, "wout", [128, 8, D], BF16)
        B_wres = Buf("wres")
        gffn = sbt(es, "gffn", [128, 8], F32)
        gfin = sbt(es, "gfin", [128, D], F32)
        cw = sbt(es, "cw", [128, 2 * NJ, 3], F32)
        cbt = sbt(es, "cbt", [128, 2 * NJ], F32)
        halo = sbt(es, "halo", [128, 2 * NJ, 2], F32)
        B_halo = Buf("halo")
        B_cc = Buf("constC")
        P.dma(lambda e: e.dma_start(out=gffn[:], in_=gffn_d), B_cc, writes=[B_cc])
        P.dma(lambda e: e.dma_start(out=gfin[:], in_=gfin_d), B_cc, writes=[B_cc])
        P.dma(lambda e: e.dma_start(out=cw[:], in_=cw_d), B_cc, writes=[B_cc])
        P.dma(lambda e: e.dma_start(out=cbt[:], in_=cb_d), B_cc, writes=[B_cc])
        P.op("vector", lambda e: e.memset(halo[:], 0.0), writes=[B_halo])


        with ExitStack() as sb:
            QA = [sbt(sb, f"QA{i}", [128, T], BF16) for i in range(2)]
            KA = [sbt(sb, f"KA{i}", [128, T], BF16) for i in range(2)]
            VA = [sbt(sb, f"VA{i}", [128, NT + 1, 65], BF16) for i in range(2)]
            VAf = [VA[i][:].rearrange("p t c -> p (t c)") for i in range(2)]
            B_QAq = [Buf(f"QAq{i}") for i in range(2)]
            B_QAs = [Buf(f"QAs{i}") for i in range(2)]
            B_KA = [Buf(f"KA{i}") for i in range(2)]
            B_VA = [Buf(f"VA{i}") for i in range(2)]
            mksb = sbt(sb, "mksb", [128, 896], BF16)
            mkmo = sbt(sb, "mkmo", [128, 896], BF16)
            pmt = sbt(sb, "pmt", [128, NT * nblk], BF16)
            umt = sbt(sb, "umt", [128, NT * nblk], BF16)
            B_cb = Buf("constB")
            e32 = [sbt(sb, f"e32_{i}", [128, 512], F32) for i in range(3)]
            B_e32 = [Buf(f"e32_{i}") for i in range(3)]
            Lb = [sbt(sb, f"Lb{i}", [128, 512], BF16) for i in range(4)]
            B_Lb = [Buf(f"Lb{i}") for i in range(4)]
            Rb = [sbt(sb, f"Rb{i}", [128, 512], BF16) for i in range(4)]
            B_Rb = [Buf(f"Rb{i}") for i in range(4)]
            wb = [sbt(sb, f"wb{i}", [128, 512], BF16) for i in range(4)]
            B_wb = [Buf(f"wb{i}") for i in range(4)]
            yst = [sbt(sb, f"yst{i}", [64, 512], BF16) for i in range(4)]
            B_yst = [Buf(f"yst{i}") for i in range(4)]
            rden = sbt(sb, "rden", [128, 512], F32)
            B_rden = Buf("rden")
            bcs = sbt(sb, "bcs", [64, 512], F32)
            B_bcs = Buf("bcs")
            km32 = sbt(sb, "km32", [64, nblk], F32)
            kmb = sbt(sb, "kmb", [128, nblk], BF16)
            B_km32 = Buf("km32")
            B_kmb = Buf("kmb")
            gm32 = sbt(sb, "gm32", [128, 16 * nblk], F32)
            B_gm32 = Buf("gm32")
            m8 = sbt(sb, "m8", [128, 16, 8], F32)
            B_m8 = Buf("m8")
            sel = sbt(sb, "sel", [128, 16 * nblk], F32)
            B_sel = Buf("sel")
            sel2 = sbt(sb, "sel2", [128, 16 * nblk], F32)
            B_sel2 = Buf("sel2")
            selT = sbt(sb, "selT", [128, 16, 96], BF16)
            B_selT = Buf("selT")

            cst = [sbt(sb, f"cst{i}", [128, 1408], F32) for i in range(2)]
            cbf = [sbt(sb, f"cbf{i}", [128, 1408], BF16) for i in range(2)]
            B_cst = [Buf(f"cst{i}") for i in range(2)]
            B_cbf = [Buf(f"cbf{i}") for i in range(2)]
            ci = [0]

            def conv_piece(src_ap, a, n, scale_ap=None, dst_fn=None, dst_sbuf_ap=None, Bdst=None):
                ncols = a * n
                b = ci[0] % 2
                ci[0] += 1
                dstv = cst[b][:, 0:ncols] if a == 1 else cst[b][:, 0:ncols].rearrange("p (a n) -> p a n", a=a)
                P.dma(lambda e: e.dma_start(out=dstv, in_=src_ap), B_cst[b], writes=[B_cst[b]])
                if dst_fn is not None:
                    if scale_ap is not None:
                        P.op("vector", lambda e: e.tensor_scalar(out=cbf[b][:, 0:ncols], in0=cst[b][:, 0:ncols],
                                                                 scalar1=scale_ap, scalar2=None, op0=ALU.mult),
                             reads=[B_cst[b], B_cc], writes=[B_cbf[b]])
                    else:
                        P.op("vector", lambda e: e.tensor_copy(out=cbf[b][:, 0:ncols], in_=cst[b][:, 0:ncols]),
                             reads=[B_cst[b]], writes=[B_cbf[b]])
                    dst_fn(b)
                else:
                    P.op("vector", lambda e: e.tensor_copy(out=dst_sbuf_ap, in_=cst[b][:, 0:ncols]),
                         reads=[B_cst[b]], writes=[Bdst])

            def conv_gen():
                for k in range(8):
                    for q4 in range(4):
                        def dst_fn(b, k=k, q4=q4):
                            j0 = q4 * 11
                            P.dma(lambda e: e.dma_start(
                                out=wup_s[j0:j0 + 11, :, k, :].rearrange("j p c -> p j c"),
                                in_=cbf[b][:, 0:1408].rearrange("p (j c) -> p j c", c=128)),
                                B_cbf[b], reads=[B_cbf[b]], writes=[B_wup])
                        conv_piece(wup_d[k * 128:(k + 1) * 128, q4 * 1408:(q4 + 1) * 1408], 1, 1408,
                                   scale_ap=gffn[:, k:k + 1], dst_fn=dst_fn)
                        yield
                for j in range(NJ):
                    def dst_fn(b, j=j):
                        P.dma(lambda e: e.dma_start(out=wdn_s[j * 128:(j + 1) * 128, :], in_=cbf[b][:, 0:1024]),
                              B_cbf[b], reads=[B_cbf[b]], writes=[B_wdn])
                    conv_piece(wdn_d[j * 128:(j + 1) * 128, :], 1, 1024, dst_fn=dst_fn)
                    yield
                for k in range(4):
                    conv_piece(wpm_d[k * 128:(k + 1) * 128, :], 1, 1024, dst_sbuf_ap=wpm[:, k, :], Bdst=B_wres)
                    yield
                    conv_piece(wps_d[k * 128:(k + 1) * 128, :], 1, 1024, dst_sbuf_ap=wps[:, k, :], Bdst=B_wres)
                    yield
                for k in range(8):
                    conv_piece(wout_d[k * 128:(k + 1) * 128, :], 1, 1024, dst_sbuf_ap=wout[:, k, :], Bdst=B_wres)
                    yield

            P.dma(lambda e: e.dma_start(out=mksb[:], in_=mksb_d), B_cb, writes=[B_cb])
            P.dma(lambda e: e.dma_start(out=mkmo[:], in_=mkmo_d), B_cb, writes=[B_cb])
            P.dma(lambda e: e.dma_start(out=pmt[:], in_=pm_d), B_cb, writes=[B_cb])
            P.dma(lambda e: e.dma_start(out=umt[:], in_=um_d), B_cb, writes=[B_cb])
            P.op("vector", lambda e: e.memset(selT[:], 0.0), writes=[B_selT])
            P.op("vector", lambda e: e.memset(kmb[:], 0.0), writes=[B_kmb])
            P.op("vector", lambda e: e.memset(rden[:], 0.0), writes=[B_rden])
            for b in range(2):
                P.op("gpsimd", lambda e, b=b: e.memset(QA[b][64:128, :], 0.0), writes=[B_QAs[b], B_QAq[b]])
                P.op("gpsimd", lambda e, b=b: e.memset(KA[b][64:128, :], 0.0), writes=[B_KA[b]])
                P.op("vector", lambda e, b=b: e.memset(VA[b][:, NT, :], 0.0), writes=[B_VA[b]])
                P.op("vector", lambda e, b=b: e.memset(VA[b][:, 0:NT, 64:65], 1.0), writes=[B_VA[b]])
                P.dma(lambda e, b=b: e.dma_start(out=KA[b][64:64 + nblk, :], in_=ind_d), B_KA[b], writes=[B_KA[b]])

            heads = [("sb", 0)] + [("moba", h) for h in range(NH)] + [("sb", h) for h in range(1, NH)]

            def load_head(hi):
                kind, h = heads[hi]
                b = hi % 2
                hg = h if kind == "moba" else 8 + h
                if kind == "sb" and hi in (NH + 1, NH + 2):
                    P.op("gpsimd", lambda e: e.memset(QA[b][64:128, :], 0.0), writes=[B_QAs[b], B_QAq[b]])
                P.dma(lambda e: e.dma_start(out=QA[b][0:64, :], in_=qt_s[hg * 64:(hg + 1) * 64, :]),
                      B_QAq[b], reads=[B_qt], writes=[B_QAq[b]])
                P.dma(lambda e: e.dma_start(out=KA[b][0:64, :], in_=kt_s[hg * 64:(hg + 1) * 64, :]),
                      B_KA[b], reads=[B_kt], writes=[B_KA[b]])
                step = 8
                for t0 in range(0, NT, step):
                    P.dma(lambda e, t0=t0: e.dma_start(
                        out=VA[b][:, t0:t0 + step, 0:64],
                        in_=v_s[hg, t0 * 128:(t0 + step) * 128, :].rearrange("(t p) d -> p t d", p=128)),
                        B_VA[b], reads=[B_v], writes=[B_VA[b]])
                if kind == "moba":
                    P.dma(lambda e: e.dma_start(out=QA[b][96:100, :], in_=qal_d[h]), B_QAq[b], writes=[B_QAq[b]])
                    P.dma(lambda e: e.dma_start(out=KA[b][96:100, :], in_=kal_d[h]), B_KA[b], writes=[B_KA[b]])

            def selection(hi):
                kind, h = heads[hi]
                b = hi % 2
                for n0 in range(0, nblk, 4):
                    P.op("vector", lambda e, n0=n0: e.tensor_reduce(
                        out=km32[:, n0:n0 + 4],
                        in_=KA[b][0:64, n0 * 256:(n0 + 4) * 256].rearrange("p (n k) -> p n k", k=256),
                        axis=AX.X, op=ALU.add),
                        reads=[B_KA[b]], writes=[B_km32])
                    yield
                P.op("vector", lambda e: e.tensor_scalar(out=kmb[0:64, :], in0=km32[:, :], scalar1=1.0 / 256, scalar2=None,
                                                         op0=ALU.mult),
                     reads=[B_km32], writes=[B_kmb])
                for q0 in range(0, NT, 16):
                    nq = min(16, NT - q0)

                    def gate_mm(e, q0=q0, nq=nq):
                        inst = None
                        for i in range(nq):
                            qt_ = q0 + i
                            inst = e.matmul(pb[6][:, i * nblk:(i + 1) * nblk], lhsT=QA[b][:, qt_ * 128:(qt_ + 1) * 128],
                                            rhs=kmb[:, :], start=True, stop=True)
                        return inst
                    P.op("tensor", gate_mm, reads=[B_QAq[b], B_QAs[b], B_kmb], writes=[PB[6]])
                    P.op("vector", lambda e, q0=q0, nq=nq: e.tensor_tensor(
                        out=gm32[:, 0:nq * nblk], in0=pb[6][:, 0:nq * nblk], in1=pmt[:, q0 * nblk:(q0 + nq) * nblk],
                        op=ALU.add),
                        reads=[PB[6], B_cb], writes=[B_gm32])
                    yield
                    for i in range(nq):
                        P.op("vector", lambda e, i=i: e.max(out=m8[:, i, :], in_=gm32[:, i * nblk:(i + 1) * nblk]),
                             reads=[B_gm32], writes=[B_m8])
                        if i % 4 == 3:
                            yield
                    for i in range(nq):
                        P.op("vector", lambda e, i=i: e.tensor_scalar(
                            out=sel[:, i * nblk:(i + 1) * nblk], in0=gm32[:, i * nblk:(i + 1) * nblk],
                            scalar1=m8[:, i, 2:3], scalar2=1.0, op0=ALU.is_ge, op1=ALU.subtract),
                            reads=[B_gm32, B_m8], writes=[B_sel])
                        if i % 4 == 3:
                            yield
                    P.op("vector", lambda e, q0=q0, nq=nq: e.scalar_tensor_tensor(
                        out=sel2[:, 0:nq * nblk], in0=sel[:, 0:nq * nblk], scalar=BIG,
                        in1=umt[:, q0 * nblk:(q0 + nq) * nblk], op0=ALU.mult, op1=ALU.add),
                        reads=[B_sel, B_cb], writes=[B_sel2])
                    P.op("vector", lambda e, nq=nq: e.tensor_scalar(
                        out=selT[:, 0:nq, 64:64 + nblk], in0=sel2[:, 0:nq * nblk].rearrange("p (i n) -> p i n", n=nblk),
                        scalar1=0.0, scalar2=None, op0=ALU.min),
                        reads=[B_sel2], writes=[B_selT])
                    yield
                    yield
                    for g0 in range(0, nq, 4):
                        def tr_mm(e, g0=g0):
                            inst = None
                            for i in range(4):
                                inst = e.matmul(pb[7][0:96, i * 128:(i + 1) * 128], lhsT=selT[:, g0 + i, 0:96],
                                                rhs=ident[:], start=True, stop=True)
                            return inst
                        P.op("tensor", tr_mm, reads=[B_selT, B_const], writes=[PB[7]])
                        c0 = (q0 + g0) * 128
                        P.op("vector", lambda e, c0=c0: e.tensor_copy(out=QA[b][64:96, c0:c0 + 512], in_=pb[7][64:96, :]),
                             reads=[PB[7]], writes=[B_QAs[b]])
                        yield

            ycnt = [0]

            def moba_head(hi, gens):
                kind, h = heads[hi]
                b = hi % 2
                units = [(c, i) for c in range(NCH) for i in range(4 * c + 4)]
                N = len(units)
                LA = 3
                pslot = {}
                pending = []
                for n in range(N + LA):
                    if n % 5 == 0 and n >= 64:
                        for g in gens:
                            next(g, None)
                    while pending and pending[0][0] <= n:
                        pending.pop(0)[1]()
                    if n < N:
                        c, i = units[n]
                        bk = n % 4
                        diag = i >= 4 * c
                        lo = 128 * (i - 4 * c) if diag else 0

                        def s1(e, c=c, i=i, bk=bk, diag=diag, lo=lo):
                            inst = e.matmul(pb[bk][:, lo:512], lhsT=KA[b][:, i * 128:(i + 1) * 128],
                                            rhs=QA[b][:, c * 512 + lo:(c + 1) * 512], start=True, stop=not diag)
                            if diag:
                                inst = e.matmul(pb[bk][:, lo:512], lhsT=ident[:], rhs=mkmo[:, 384:384 + 512 - lo],
                                                start=False, stop=True)
                            return inst
                        P.op("tensor", s1, reads=[B_KA[b], B_QAq[b], B_QAs[b], B_const, B_cb], writes=[PB[bk]])
                        ps_ = n % 4
                        pslot[n] = ps_
                        P.op("scalar", lambda e, bk=bk, ps_=ps_, lo=lo: e.activation(
                            out=wb[ps_][:, lo:512], in_=pb[bk][:, lo:512], func=AF.Exp),
                            reads=[PB[bk]], writes=[B_wb[ps_]])
                    m = n - LA
                    if m >= 0:
                        c, i = units[m]
                        yb = 4 + (c % 2)
                        ps_ = pslot[m]
                        last = (i == 4 * c + 3)
                        lo = 128 * (i - 4 * c) if i >= 4 * c else 0
                        P.op("tensor", lambda e, c=c, i=i, yb=yb, ps_=ps_, last=last, lo=lo: e.matmul(
                            pb[yb][:, lo:512], lhsT=VAf[b][:, i * 65:i * 65 + 128], rhs=wb[ps_][:, lo:512],
                            start=(i == 0), stop=last, skip_group_check=True),
                            reads=[B_VA[b], B_wb[ps_]], writes=[PB[yb]])
                        if last:
                            while pending:
                                pending.pop(0)[1]()
                            ys = ycnt[0] % 4
                            ycnt[0] += 1
                            P.op("vector", lambda e, yb=yb: e.reciprocal(out=rden[64:65, :], in_=pb[yb][64:65, :]),
                                 reads=[PB[yb]], writes=[B_rden])

                            def finish(yb=yb, ys=ys, c=c):
                                P.op("tensor", lambda e: e.matmul(pb[6][:, :], lhsT=ones32[:, :], rhs=rden[:, :],
                                                                  start=True, stop=True),
                                     reads=[B_rden, B_const], writes=[PB[6]])
                                P.op("vector", lambda e: e.tensor_copy(out=bcs[:, :], in_=pb[6][0:64, :]),
                                     reads=[PB[6]], writes=[B_bcs])
                                P.op("vector", lambda e: e.tensor_tensor(
                                    out=yst[ys][:, :], in0=pb[yb][0:64, :], in1=bcs[:, :], op=ALU.mult),
                                    reads=[PB[yb], B_bcs], writes=[B_yst[ys]])
                                P.dma(lambda e: e.dma_start(
                                    out=y_s[h * 64:(h + 1) * 64, c * 512:(c + 1) * 512], in_=yst[ys][:, :]),
                                    B_yst[ys], reads=[B_yst[ys]], writes=[B_y])
                            pending.append((n + 10, finish))
                while pending:
                    pending.pop(0)[1]()
                for g in gens:
                    if g is not conv_g[0]:
                        for _ in g:
                            pass

            def sb_head(hi, gens):
                kind, h = heads[hi]
                b = hi % 2
                units = [(c, i) for c in range(NCH) for i in range(4 * c + 3, -1, -1)]
                N = len(units)
                info = {}
                rcnt = [0]

                def S1(n):
                    c, i = units[n]
                    bk = n % 4
                    diag = i >= 4 * c
                    first = (i == 4 * c + 3)
                    lastu = (i == 0)
                    lo = 128 * (i - 4 * c) if diag else 0
                    info[n] = dict(c=c, i=i, bk=bk, first=first, last=lastu, e=n % 3, L=n % 4, w=n % 4, lo=lo)

                    def s1(e):
                        inst = e.matmul(pb[bk][:, lo:512], lhsT=KA[b][:, i * 128:(i + 1) * 128],
                                        rhs=QA[b][:, c * 512 + lo:(c + 1) * 512], start=True, stop=not diag)
                        if diag:
                            inst = e.matmul(pb[bk][:, lo:512], lhsT=ident[:], rhs=mksb[:, 384:384 + 512 - lo],
                                            start=False, stop=True)
                        return inst
                    P.op("tensor", s1, reads=[B_KA[b], B_QAq[b], B_QAs[b], B_const, B_cb], writes=[PB[bk]])

                def A1(n):
                    d = info[n]
                    lo = d["lo"]
                    P.op("scalar", lambda e: e.activation(out=e32[d["e"]][:, lo:512], in_=pb[d["bk"]][:, lo:512], func=AF.Exp),
                         reads=[PB[d["bk"]]], writes=[B_e32[d["e"]]])

                def A2(n):
                    d = info[n]
                    lo = d["lo"]
                    P.op("scalar", lambda e: e.activation(out=Lb[d["L"]][:, lo:512], in_=e32[d["e"]][:, lo:512], func=AF.Ln,
                                                          bias=1.0, scale=1.0),
                         reads=[B_e32[d["e"]]], writes=[B_Lb[d["L"]]])

                def S3(n):
                    d = info[n]
                    if d["last"]:
                        return
                    lo = d["lo"]
                    rcnt[0] += 1
                    rn = rcnt[0] % 4
                    d["rn"] = rn
                    if d["first"]:
                        P.op("gpsimd", lambda e: e.tensor_copy(out=Rb[rn][:, lo:512], in_=Lb[d["L"]][:, lo:512]),
                             reads=[B_Lb[d["L"]]], writes=[B_Rb[rn]])
                    else:
                        rp = info[n - 1]["rn"]
                        lop = info[n - 1]["lo"]
                        if lop > lo:
                            P.op("gpsimd", lambda e: e.tensor_copy(out=Rb[rn][:, lo:lop], in_=Lb[d["L"]][:, lo:lop]),
                                 reads=[B_Lb[d["L"]]], writes=[B_Rb[rn]])
                        P.op("gpsimd", lambda e: e.tensor_tensor(out=Rb[rn][:, lop:512], in0=Rb[rp][:, lop:512],
                                                                 in1=Lb[d["L"]][:, lop:512], op=ALU.add),
                             reads=[B_Lb[d["L"]], B_Rb[rp]], writes=[B_Rb[rn]])

                def S4(n):
                    d = info[n]
                    lo = d["lo"]
                    rds = [B_Lb[d["L"]], B_const]
                    rp = None
                    lop = 0
                    if not d["first"]:
                        rp = info[n - 1]["rn"]
                        lop = info[n - 1]["lo"]
                        rds.append(B_Rb[rp])

                    def s4(e):
                        inst = e.matmul(pb[d["bk"]][:, lo:512], lhsT=negtri[:], rhs=Lb[d["L"]][:, lo:512], start=False,
                                        stop=d["first"], skip_group_check=True)
                        if rp is not None:
                            inst = e.matmul(pb[d["bk"]][:, lop:512], lhsT=negones[:], rhs=Rb[rp][:, lop:512], start=False,
                                            stop=True, skip_group_check=True)
                        return inst
                    P.op("tensor", s4, reads=rds, writes=[PB[d["bk"]]])

                def A3(n):
                    d = info[n]
                    lo = d["lo"]
                    P.op("scalar", lambda e: e.activation(out=wb[d["w"]][:, lo:512], in_=pb[d["bk"]][:, lo:512], func=AF.Exp),
                         reads=[PB[d["bk"]]], writes=[B_wb[d["w"]]])

                def S6(n):
                    d = info[n]
                    c, i = d["c"], d["i"]
                    lo = d["lo"]
                    yb = 4 + (c % 2)
                    P.op("tensor", lambda e: e.matmul(pb[yb][:, lo:512], lhsT=VAf[b][:, i * 65:i * 65 + 128],
                                                      rhs=wb[d["w"]][:, lo:512],
                                                      start=d["first"], stop=d["last"], skip_group_check=True),
                         reads=[B_VA[b], B_wb[d["w"]]], writes=[PB[yb]])
                    if d["last"]:
                        ys = ycnt[0] % 4
                        ycnt[0] += 1
                        P.op("vector", lambda e: e.tensor_copy(out=yst[ys][:, :], in_=pb[yb][0:64, :]),
                             reads=[PB[yb]], writes=[B_yst[ys]])
                        P.dma(lambda e: e.dma_start(
                            out=y_s[512 + h * 64:512 + (h + 1) * 64, c * 512:(c + 1) * 512], in_=yst[ys][:, :]),
                            B_yst[ys], reads=[B_yst[ys]], writes=[B_y])

                for n in range(N + 3):
                    if n % 5 == 0:
                        for g in gens:
                            next(g, None)
                    if n < N:
                        S1(n)
                        A1(n)
                    if 0 <= n - 1 < N:
                        S4(n - 1)
                    if 0 <= n - 2 < N:
                        A3(n - 2)
                    if 0 <= n - 3 < N:
                        S6(n - 3)
                    if n < N:
                        A2(n)
                        S3(n)
                for g in gens:
                    if g is not conv_g[0]:
                        for _ in g:
                            pass

            conv_g = [conv_gen()]
            load_head(0)
            for hi in range(len(heads)):
                gens = []
                if hi + 1 < len(heads):
                    load_head(hi + 1)
                    if heads[hi + 1][0] == "moba":
                        gens.append(selection(hi + 1))
                if heads[hi][0] == "moba":
                    moba_head(hi, gens)
                else:
                    gens.append(conv_g[0])
                    sb_head(hi, gens)
            for _ in conv_g[0]:
                pass
            P.barrier()
            P.emit()

        with ExitStack() as sc:
            wup = [sbt(sc, f"wup{i}", [128, 8, 256], BF16) for i in range(3)]
            B_wupb = [Buf(f"wup{i}") for i in range(3)]
            wdn = [sbt(sc, f"wdn{i}", [128, D], BF16) for i in range(4)]
            B_wdnb = [Buf(f"wdn{i}") for i in range(4)]
            YT = [sbt(sc, f"YT{i}", [128, 8, 512], BF16) for i in range(2)]
            B_YT = [Buf(f"YT{i}") for i in range(2)]
            G = [sbt(sc, f"G{i}", [128, 2, 512], BF16) for i in range(4)]
            B_G = [Buf(f"G{i}") for i in range(4)]
            xt = [sbt(sc, f"xc{i}", [128, D], F32) for i in range(8)]
            B_xt = [Buf(f"xc{i}") for i in range(8)]
            t1 = [sbt(sc, f"t1_{i}", [128, 512], F32) for i in range(2)]
            t2 = [sbt(sc, f"t2_{i}", [128, 512], F32) for i in range(2)]
            B_t1 = [Buf(f"t1_{i}") for i in range(2)]
            B_t2 = [Buf(f"t2_{i}") for i in range(2)]
            mixT = [sbt(sc, f"mixT{m}", [128, 8, 512], BF16) for m in range(2)]
            B_mixT = [[Buf(f"mixT{m}_{i}") for i in range(8)] for m in range(2)]
            junk = sbt(sc, "junkc", [128, D], BF16)
            B_junk = Buf("junkc")
            ss = sbt(sc, "ssc", [128, 4], F32)
            ms = sbt(sc, "msc", [128, 4], F32)
            rstd = sbt(sc, "rstdc", [128, 4], F32)
            B_ss, B_ms, B_rstd = Buf("ssc"), Buf("msc"), Buf("rstdc")
            ss2 = sbt(sc, "ss2", [128, 4], F32)
            ms2 = sbt(sc, "ms2", [128, 4], F32)
            rstd2 = sbt(sc, "rstd2", [128, 4], F32)
            B_ss2, B_ms2, B_rstd2 = Buf("ss2"), Buf("ms2"), Buf("rstd2")
            hn = [sbt(sc, f"hnc{i}", [128, D], BF16) for i in range(4)]
            B_hn = [Buf(f"hnc{i}") for i in range(4)]
            h2T = sbt(sc, "h2T", [128, 8, 512], BF16)
            B_h2T = Buf("h2T")
            mT = sbt(sc, "mT", [128, NJ, 512], BF16)
            B_mT = [Buf(f"mT{i}") for i in range(NJ)]
            raw = [sbt(sc, f"raw{i}", [128, 514], F32) for i in range(3)]
            B_raw = [Buf(f"raw{i}") for i in range(3)]
            tt = [sbt(sc, f"tt{i}", [128, 512], F32) for i in range(3)]
            B_tt = [Buf(f"tt{i}") for i in range(3)]
            uu = [sbt(sc, f"uu{i}", [128, 512], F32) for i in range(4)]
            B_uu = [Buf(f"uu{i}") for i in range(4)]
            sg = [sbt(sc, f"sg{i}", [128, 512], F32) for i in range(2)]
            B_sg = [Buf(f"sg{i}") for i in range(2)]
            pbTs = [pb[6][:].bitcast(BF16), pb[7][:].bitcast(BF16)]

            def load_x(c):
                for s in range(4):
                    xi = (c % 2) * 4 + s
                    t0 = c * 512 + s * 128
                    P.dma(lambda e, xi=xi, t0=t0: e.dma_start(out=xt[xi][:], in_=x_d[t0:t0 + 128, :]),
                          B_xt[xi], writes=[B_xt[xi]])

            def load_YT(c):
                yb_ = c % 2
                t0c = c * 512
                P.dma(lambda e: e.dma_start(
                    out=YT[yb_][:], in_=y_s[:, t0c:t0c + 512].rearrange("(k p) t -> p k t", p=128)),
                    B_YT[yb_], reads=[B_y], writes=[B_YT[yb_]])

            g_iss = [0]

            def ensure_G(upto):
                while g_iss[0] <= min(upto, NCH * 8 - 1):
                    idx = g_iss[0]
                    g_iss[0] += 1
                    c_, f_ = idx // 8, idx % 8
                    gb = idx % 4
                    t0c = c_ * 512
                    P.dma(lambda e, gb=gb, f_=f_, t0c=t0c: e.dma_start(
                        out=G[gb][:], in_=g_s[:, t0c:t0c + 512].rearrange("(a r) t -> r a t", a=2)[f_ * 128:(f_ + 1) * 128]),
                        B_G[gb], reads=[B_g], writes=[B_G[gb]])

            u_iss = [0]

            def ensure_wup(upto):
                while u_iss[0] <= min(upto, NCH * NJ - 1):
                    idx = u_iss[0]
                    u_iss[0] += 1
                    j_ = idx % NJ
                    wb_ = idx % 3
                    P.dma(lambda e, wb_=wb_, j_=j_: e.dma_start(out=wup[wb_][:, :, 0:128], in_=wup_s[j_]),
                          B_wupb[wb_], reads=[B_wup], writes=[B_wupb[wb_]])
                    P.dma(lambda e, wb_=wb_, j_=j_: e.dma_start(out=wup[wb_][:, :, 128:256], in_=wup_s[NJ + j_]),
                          B_wupb[wb_], reads=[B_wup], writes=[B_wupb[wb_]])

            d_iss = [0]

            def ensure_wdn(upto):
                while d_iss[0] <= min(upto, NCH * 2 * NJ - 1):
                    idx = d_iss[0]
                    d_iss[0] += 1
                    j_ = idx % NJ
                    db = idx % 4
                    P.dma(lambda e, db=db, j_=j_: e.dma_start(out=wdn[db][:], in_=wdn_s[j_ * 128:(j_ + 1) * 128, :]),
                          B_wdnb[db], reads=[B_wdn], writes=[B_wdnb[db]])

            load_x(0)
            load_YT(0)
            ensure_G(1)
            bki = [0]

            def nextbank():
                bk = bki[0] % 6
                bki[0] += 1
                return bk

            ri = [0]
            oi = [0]
            prev_silu = [None]

            def P1(c):
                t0c = c * 512
                ycb = c % 2
                mb = c % 2
                for f in range(8):
                    gb = (c * 8 + f) % 4
                    ensure_G(c * 8 + f + 2)
                    bka = nextbank()
                    bkb = nextbank()

                    def mma(e, f=f, bka=bka, ycb=ycb):
                        inst = None
                        for k in range(4):
                            inst = e.matmul(pb[bka][:], lhsT=wpm[:, k, f * 128:(f + 1) * 128], rhs=YT[ycb][:, k, :],
                                            start=(k == 0), stop=(k == 3))
                        return inst

                    def mmb(e, f=f, bkb=bkb, ycb=ycb):
                        inst = None
                        for k in range(4):
                            inst = e.matmul(pb[bkb][:], lhsT=wps[:, k, f * 128:(f + 1) * 128], rhs=YT[ycb][:, 4 + k, :],
                                            start=(k == 0), stop=(k == 3))
                        return inst
                    P.op("tensor", mma, reads=[B_wres, B_YT[ycb]], writes=[PB[bka]])
                    P.op("tensor", mmb, reads=[B_wres, B_YT[ycb]], writes=[PB[bkb]])
                    tb = f % 2
                    P.op("vector", lambda e, bka=bka, gb=gb, tb=tb: e.tensor_tensor(
                        out=t1[tb][:], in0=pb[bka][:], in1=G[gb][:, 0, :], op=ALU.mult),
                        reads=[PB[bka], B_G[gb]], writes=[B_t1[tb]])
                    P.op("vector", lambda e, bkb=bkb, gb=gb, tb=tb: e.tensor_tensor(
                        out=t2[tb][:], in0=pb[bkb][:], in1=G[gb][:, 1, :], op=ALU.mult),
                        reads=[PB[bkb], B_G[gb]], writes=[B_t2[tb]])
                    P.op("gpsimd", lambda e, f=f, tb=tb, mb=mb: e.tensor_tensor(
                        out=mixT[mb][:, f, :], in0=t1[tb][:], in1=t2[tb][:], op=ALU.add),
                        reads=[B_t1[tb], B_t2[tb]], writes=[B_mixT[mb][f]])

            def P2(c):
                mb = c % 2
                for s in range(4):
                    xi = (c % 2) * 4 + s
                    for hf in range(2):
                        bk = nextbank()

                        def mmo(e, s=s, hf=hf, bk=bk, mb=mb):
                            inst = None
                            for f in range(8):
                                inst = e.matmul(pb[bk][:], lhsT=mixT[mb][:, f, s * 128:(s + 1) * 128],
                                                rhs=wout[:, f, hf * 512:(hf + 1) * 512], start=(f == 0), stop=(f == 7))
                            return inst
                        P.op("tensor", mmo, reads=B_mixT[mb] + [B_wres], writes=[PB[bk]])
                        P.op("vector", lambda e, xi=xi, hf=hf, bk=bk: e.tensor_tensor(
                            out=xt[xi][:, hf * 512:(hf + 1) * 512], in0=pb[bk][:], in1=xt[xi][:, hf * 512:(hf + 1) * 512],
                            op=ALU.add),
                            reads=[PB[bk]], writes=[B_xt[xi]])
                    P.op("scalar", lambda e, xi=xi, s=s: e.activation(
                        out=junk[:], in_=xt[xi][:], func=AF.Square, accum_out=ss[:, s:s + 1]),
                        reads=[B_xt[xi]], writes=[B_junk, B_ss])

            def P3a(c):
                rstd_from_ss(ss, ms, rstd, B_ss, B_ms, B_rstd, 4)
                for s in range(4):
                    xi = (c % 2) * 4 + s
                    hb = s
                    P.op("vector", lambda e, xi=xi, s=s, hb=hb: e.tensor_scalar(
                        out=hn[hb][:], in0=xt[xi][:], scalar1=rstd[:, s:s + 1], scalar2=None, op0=ALU.mult),
                        reads=[B_xt[xi], B_rstd], writes=[B_hn[hb]])

            def P3b(c):
                for s in range(4):
                    hb = s
                    pbi = s % 2

                    def tr(e, hb=hb, pbi=pbi):
                        inst = None
                        for j in range(8):
                            inst = e.transpose(out=pbTs[pbi][:, j * 128:(j + 1) * 128], in_=hn[hb][:, j * 128:(j + 1) * 128],
                                               identity=ident[:])
                        return inst
                    P.op("tensor", tr, reads=[B_hn[hb], B_const], writes=[PB[6 + pbi]])
                    P.op("scalar", lambda e, s=s, pbi=pbi: e.activation(
                        out=h2T[:, :, s * 128:(s + 1) * 128], in_=pbTs[pbi].rearrange("p (j t) -> p j t", j=8), func=AF.Copy),
                        reads=[PB[6 + pbi]], writes=[B_h2T])

            def P4(c):
                for j in range(NJ):
                    wb_ = (c * NJ + j) % 3
                    ensure_wup(c * NJ + j + 2)
                    if j == NJ - 3:
                        ensure_wdn(c * 2 * NJ + 3)
                    res = []
                    for ab in range(2):
                        jj = ab * NJ + j
                        bk = nextbank()

                        def mmu(e, wb_=wb_, ab=ab, bk=bk):
                            inst = None
                            for k in range(8):
                                inst = e.matmul(pb[bk][:], lhsT=wup[wb_][:, k, ab * 128:(ab + 1) * 128], rhs=h2T[:, k, :],
                                                start=(k == 0), stop=(k == 7))
                            return inst
                        P.op("tensor", mmu, reads=[B_wupb[wb_], B_h2T], writes=[PB[bk]])
                        rb = ri[0] % 3
                        ub = ri[0] % 4
                        ri[0] += 1
                        P.op("scalar", lambda e, rb=rb, bk=bk: e.activation(out=raw[rb][:, 2:514], in_=pb[bk][:], func=AF.Copy),
                             reads=[PB[bk]], writes=[B_raw[rb]])
                        P.op("scalar", lambda e, rb=rb, bk=bk, jj=jj: e.activation(
                            out=tt[rb][:], in_=pb[bk][:], func=AF.Identity, scale=cw[:, jj, 2:3], bias=cbt[:, jj:jj + 1]),
                            reads=[PB[bk], B_cc], writes=[B_tt[rb]])
                        P.op("gpsimd", lambda e, rb=rb, jj=jj: e.tensor_copy(out=raw[rb][:, 0:2], in_=halo[:, jj, :]),
                             reads=[B_halo], writes=[B_raw[rb]])
                        P.op("gpsimd", lambda e, rb=rb, jj=jj: e.tensor_copy(out=halo[:, jj, :], in_=raw[rb][:, 512:514]),
                             reads=[B_raw[rb]], writes=[B_halo])
                        P.op("vector", lambda e, rb=rb, jj=jj: e.scalar_tensor_tensor(
                            out=tt[rb][:], in0=raw[rb][:, 1:513], scalar=cw[:, jj, 1:2], in1=tt[rb][:],
                            op0=ALU.mult, op1=ALU.add),
                            reads=[B_raw[rb], B_cc], writes=[B_tt[rb]])
                        P.op("vector", lambda e, rb=rb, jj=jj, ub=ub: e.scalar_tensor_tensor(
                            out=uu[ub][:], in0=raw[rb][:, 0:512], scalar=cw[:, jj, 0:1], in1=tt[rb][:],
                            op0=ALU.mult, op1=ALU.add),
                            reads=[B_raw[rb], B_tt[rb], B_cc], writes=[B_uu[ub]])
                        res.append(ub)
                    if prev_silu[0] is not None:
                        prev_silu[0]()

                    def do_silu(j=j, res=tuple(res)):
                        sgb = j % 2
                        P.op("scalar", lambda e: e.activation(out=sg[sgb][:], in_=uu[res[0]][:], func=AF.Silu),
                             reads=[B_uu[res[0]]], writes=[B_sg[sgb]])
                        P.op("gpsimd", lambda e: e.tensor_tensor(
                            out=mT[:, j, :], in0=sg[sgb][:], in1=uu[res[1]][:], op=ALU.mult),
                            reads=[B_sg[sgb], B_uu[res[1]]], writes=[B_mT[j]])
                    prev_silu[0] = do_silu
                prev_silu[0]()
                prev_silu[0] = None

            def P5(c):
                for sp2 in range(2):
                    bks = [[nextbank() for hf in range(2)] for s in range(2)]
                    for j in range(NJ):
                        didx = (c * 2 + sp2) * NJ + j
                        db = didx % 4
                        ensure_wdn(didx + 3)

                        def mmd(e, j=j, db=db, bks=bks, sp2=sp2):
                            inst = None
                            for s in range(2):
                                for hf in range(2):
                                    tok = (sp2 * 2 + s) * 128
                                    inst = e.matmul(pb[bks[s][hf]][:], lhsT=mT[:, j, tok:tok + 128],
                                                    rhs=wdn[db][:, hf * 512:(hf + 1) * 512], start=(j == 0),
                                                    stop=(j == NJ - 1))
                            return inst
                        P.op("tensor", mmd, reads=[B_mT[j], B_wdnb[db]], writes=[PB[bks[s][hf]] for s in range(2) for hf in range(2)])
                    for s in range(2):
                        sidx = sp2 * 2 + s
                        xi = (c % 2) * 4 + sidx
                        for hf in range(2):
                            bk = bks[s][hf]
                            P.op("vector", lambda e, xi=xi, hf=hf, bk=bk: e.tensor_tensor(
                                out=xt[xi][:, hf * 512:(hf + 1) * 512], in0=pb[bk][:], in1=xt[xi][:, hf * 512:(hf + 1) * 512],
                                op=ALU.add),
                                reads=[PB[bk]], writes=[B_xt[xi]])
                        P.op("scalar", lambda e, xi=xi, sidx=sidx: e.activation(
                            out=junk[:], in_=xt[xi][:], func=AF.Square, accum_out=ss2[:, sidx:sidx + 1]),
                            reads=[B_xt[xi]], writes=[B_junk, B_ss2])

            def F1(c):
                rstd_from_ss(ss2, ms2, rstd2, B_ss2, B_ms2, B_rstd2, 4)

            def F2(c):
                for s in range(4):
                    xi = (c % 2) * 4 + s
                    P.op("vector", lambda e, xi=xi, s=s: e.scalar_tensor_tensor(
                        out=xt[xi][:], in0=xt[xi][:], scalar=rstd2[:, s:s + 1], in1=gfin[:], op0=ALU.mult, op1=ALU.mult),
                        reads=[B_rstd2, B_cc], writes=[B_xt[xi]])
                    tok = c * 512 + s * 128
                    P.dma(lambda e, xi=xi, tok=tok: e.dma_start(out=out_d[tok:tok + 128, :], in_=xt[xi][:]),
                          B_xt[xi], reads=[B_xt[xi]], writes=[B_out], eng="scalar")

            if NCH > 1:
                load_YT(1)
            P1(0)
            for c in range(NCH):
                ensure_wup(c * NJ + 1)
                P2(c)
                if c > 0:
                    F2(c - 1)
                if c + 1 < NCH:
                    load_x(c + 1)
                P3a(c)
                if c + 1 < NCH:
                    P1(c + 1)
                P3b(c)
                if c + 2 < NCH:
                    load_YT(c + 2)
                    ensure_G((c + 2) * 8 + 1)
                P4(c)
                P5(c)
                F1(c)
            F2(NCH - 1)
            P.barrier()
            P.emit()
    return nc


_CACHE = {}


def _prep_shared(w_in, w_proj_moba, w_proj_sb, w_out, w_up, w_down, g_mix, g_ffn, g_final, conv_w, conv_b, T):
    f = np.float32
    d = {}
    d["w_in"] = np.ascontiguousarray(np.asarray(w_in, f)[0])
    d["w_pm"] = np.ascontiguousarray(np.asarray(w_proj_moba, f)[0])
    d["w_ps"] = np.ascontiguousarray(np.asarray(w_proj_sb, f)[0])
    d["w_out"] = np.ascontiguousarray(np.asarray(w_out, f)[0])
    d["w_up"] = np.ascontiguousarray(np.asarray(w_up, f)[0])
    d["w_down"] = np.ascontiguousarray(np.asarray(w_down, f)[0])
    d["g_mix"] = np.ascontiguousarray(np.asarray(g_mix, f)[0].reshape(8, 128).T)
    d["g_ffn"] = np.ascontiguousarray(np.asarray(g_ffn, f)[0].reshape(8, 128).T)
    d["g_fin"] = np.ascontiguousarray(np.broadcast_to(np.asarray(g_final, f).reshape(1, D), (128, D)))
    cw = np.asarray(conv_w, f)[0, :, 0, :]
    d["cw"] = np.ascontiguousarray(cw.reshape(3, 2 * NJ, 128).transpose(2, 1, 0))
    d["cb"] = np.ascontiguousarray(np.asarray(conv_b, f)[0].reshape(2 * NJ, 128).T)
    d.update(make_consts(T))
    return d


def kernel(x, g_mix, w_in, w_proj_moba, w_proj_sb, w_out, g_ffn, w_up, conv_w, conv_b, w_down, g_final):
    x = np.asarray(x, np.float32)
    B, T, _ = x.shape
    if T not in _CACHE:
        _CACHE[T] = build(T)
    nc = _CACHE[T]
    shared = _prep_shared(w_in, w_proj_moba, w_proj_sb, w_out, w_up, w_down, g_mix, g_ffn, g_final, conv_w, conv_b, T)
    in_maps = []
    for b in range(B):
        m = dict(shared)
        m["x"] = np.ascontiguousarray(x[b])
        in_maps.append(m)
    res = run_bass_kernel_spmd(nc, in_maps, core_ids=list(range(B)))
    out = np.stack([np.asarray(r["out"], np.float32) for r in res.results], axis=0)
    return out
```
